# Optimizing a Trainium2 kernel written in Bass

```python
import math
import jax, jax.numpy as jnp
from jax import lax
import numpy as np

D_MODEL = 1024
BATCH = 32
SEQ = 2048
DEPTH = 1

D_MIX = D_MODEL
D_A = D_MIX // 2
D_B = D_MIX - D_A
A_GROUPS = 8
A_GROUP_DIM = D_A // A_GROUPS
CHUNK = 128
B_HEADS = 8
HEAD_DIM = D_B // B_HEADS
DILATED_BRANCHES = ((128, 1), (512, 4), (2048, 16))
ROPE_THETA = 10000.0
D_IN = 2 * D_A + 3 * D_B
N_KEYS = 128
N_EXPERTS = N_KEYS * N_KEYS
PEER_HEADS = 8
PEER_TOPK = 16
D_KEY = 256
PEER_TOKEN_BLOCK = 128
EPS = 1e-6

kernel_name = "hybrid_gmlp_dilated_attn_peer_block"


def rms_norm(x, g):
    xf = x.astype(jnp.float32)
    y = xf * lax.rsqrt(jnp.mean(xf * xf, axis=-1, keepdims=True) + EPS)
    return (y * g.astype(jnp.float32)).astype(x.dtype)


def layer_norm(x, g, b):
    xf = x.astype(jnp.float32)
    mu = jnp.mean(xf, axis=-1, keepdims=True)
    var = jnp.mean(jnp.square(xf - mu), axis=-1, keepdims=True)
    y = (xf - mu) * lax.rsqrt(var + EPS)
    return (y * g.astype(jnp.float32) + b.astype(jnp.float32)).astype(x.dtype)


def rope_tables(seq):
    pos = jnp.arange(seq, dtype=jnp.float32)
    inv = 1.0 / (ROPE_THETA ** (jnp.arange(0, HEAD_DIM, 2, dtype=jnp.float32) / HEAD_DIM))
    ang = pos[:, None] * inv[None, :]
    return jnp.cos(ang)[:, None, :], jnp.sin(ang)[:, None, :]


def apply_rope(t, cos, sin):
    tf = t.astype(jnp.float32)
    t1, t2 = tf[..., :HEAD_DIM // 2], tf[..., HEAD_DIM // 2:]
    return jnp.concatenate([t1 * cos - t2 * sin, t2 * cos + t1 * sin], axis=-1).astype(t.dtype)


def spatial_gating(u, v, ln_g, ln_b, w_s, b_s):
    B, S, _ = u.shape
    v = layer_norm(v, ln_g, ln_b)
    vc = v.reshape(B, S // CHUNK, CHUNK, A_GROUPS, A_GROUP_DIM)
    mixed = jnp.einsum('gpq,bnqgc->bnpgc', w_s, vc) + b_s.T[None, None, :, :, None]
    return u * mixed.reshape(B, S, D_A)


def to_residue(t, d):
    B, S = t.shape[:2]
    t = t.reshape((B, S // d, d) + t.shape[2:])
    t = jnp.swapaxes(t, 1, 2)
    return t.reshape((B * d, S // d) + t.shape[3:])


def from_residue(t, d, B):
    L = t.shape[1]
    t = t.reshape((B, d, L) + t.shape[2:])
    t = jnp.swapaxes(t, 1, 2)
    return t.reshape((B, L * d) + t.shape[3:])


def banded_attention(q, k, v, half_window):
    N, L, H, hd = q.shape
    blk = half_window
    nb = -(-L // blk)
    pad = nb * blk - L
    qb = jnp.pad(q, ((0, 0), (0, pad), (0, 0), (0, 0))).reshape(N, nb, blk, H, hd)
    kp = jnp.pad(k, ((0, 0), (blk, pad + blk), (0, 0), (0, 0))).reshape(N, nb + 2, blk, H, hd)
    vp = jnp.pad(v, ((0, 0), (blk, pad + blk), (0, 0), (0, 0))).reshape(N, nb + 2, blk, H, hd)
    kw = jnp.concatenate([kp[:, :-2], kp[:, 1:-1], kp[:, 2:]], axis=2)
    vw = jnp.concatenate([vp[:, :-2], vp[:, 1:-1], vp[:, 2:]], axis=2)
    s = jnp.einsum('nbqhd,nbkhd->nbhqk', qb, kw).astype(jnp.float32) * (HEAD_DIM ** -0.5)
    blocks = jnp.arange(nb)[:, None] * blk
    qpos = blocks + jnp.arange(blk)[None, :]
    kpos = blocks - blk + jnp.arange(3 * blk)[None, :]
    rel = kpos[:, None, :] - qpos[:, :, None]
    valid = (jnp.abs(rel) <= half_window) & (kpos >= 0)[:, None, :] & (kpos < L)[:, None, :]
    s = jnp.where(valid[None, :, None], s, -jnp.inf)
    m = jnp.max(s, axis=-1, keepdims=True)
    p = jnp.exp(s - m)
    l = jnp.sum(p, axis=-1)
    o = jnp.einsum('nbhqk,nbkhd->nbqhd', p, vw.astype(jnp.float32))
    l_t = jnp.swapaxes(l, 2, 3)
    o = o / l_t[..., None]
    lse = jnp.swapaxes(m[..., 0], 2, 3) + jnp.log(l_t)
    o = o.reshape(N, nb * blk, H, hd)[:, :L]
    lse = lse.reshape(N, nb * blk, H)[:, :L]
    return o, lse


def dilated_attention(q, k, v):
    B = q.shape[0]
    outs, lses = [], []
    for window, dil in DILATED_BRANCHES:
        half_window = window // (2 * dil)
        o, lse = banded_attention(to_residue(q, dil), to_residue(k, dil), to_residue(v, dil), half_window)
        outs.append(from_residue(o, dil, B))
        lses.append(from_residue(lse, dil, B))
    w = jax.nn.softmax(jnp.stack(lses, axis=0), axis=0)
    return jnp.sum(w[..., None] * jnp.stack(outs, axis=0), axis=0)


def peer(xn, w_query, sub_keys, expert_u, expert_v):
    B, S, D = xn.shape
    xt = xn.reshape((B * S) // PEER_TOKEN_BLOCK, PEER_TOKEN_BLOCK, D)

    def block(xb):
        q = (xb @ w_query).reshape(PEER_TOKEN_BLOCK, PEER_HEADS, 2, D_KEY // 2)
        s = jnp.einsum('thpc,pkc->thpk', q, sub_keys).astype(jnp.float32)
        s1, i1 = lax.top_k(s[:, :, 0], PEER_TOPK)
        s2, i2 = lax.top_k(s[:, :, 1], PEER_TOPK)
        cand = (s1[..., :, None] + s2[..., None, :]).reshape(PEER_TOKEN_BLOCK, PEER_HEADS, PEER_TOPK * PEER_TOPK)
        sc, ci = lax.top_k(cand, PEER_TOPK)
        e = (jnp.take_along_axis(i1, ci // PEER_TOPK, axis=-1) * N_KEYS
             + jnp.take_along_axis(i2, ci % PEER_TOPK, axis=-1))
        g = jax.nn.softmax(sc, axis=-1)
        u = expert_u[e]
        act = jax.nn.gelu(jnp.einsum('td,thkd->thk', xb, u).astype(jnp.float32))
        return jnp.einsum('thk,thkd->td', (g * act).astype(xb.dtype), expert_v[e])

    return lax.map(block, xt).reshape(B, S, D)


def setup_inputs(seed: int = 0) -> dict:
    key = jax.random.key(seed)
    ks = jax.random.split(key, 17)
    f32 = jnp.float32
    n = lambda k, shape: jax.random.normal(k, shape, dtype=f32)
    return {
        "x": n(ks[0], (BATCH, SEQ, D_MODEL)),
        "norm1_g": 1.0 + 0.02 * n(ks[1], (DEPTH, D_MODEL)),
        "w_in": n(ks[2], (DEPTH, D_MODEL, D_IN)) * D_MODEL ** -0.5,
        "ln_v_g": 1.0 + 0.02 * n(ks[3], (DEPTH, D_A)),
        "ln_v_b": 0.02 * n(ks[4], (DEPTH, D_A)),
        "w_spatial": n(ks[5], (DEPTH, A_GROUPS, CHUNK, CHUNK)) * CHUNK ** -0.5,
        "b_spatial": 0.02 * n(ks[6], (DEPTH, A_GROUPS, CHUNK)),
        "out_norm_a_g": 1.0 + 0.02 * n(ks[7], (DEPTH, D_A)),
        "out_norm_b_g": 1.0 + 0.02 * n(ks[8], (DEPTH, D_B)),
        "w_out": n(ks[9], (DEPTH, D_MIX, D_MODEL)) * D_MIX ** -0.5,
        "norm2_g": 1.0 + 0.02 * n(ks[10], (DEPTH, D_MODEL)),
        "w_query": n(ks[11], (DEPTH, D_MODEL, PEER_HEADS * D_KEY)) * D_MODEL ** -0.5,
        "sub_keys": n(ks[12], (DEPTH, 2, N_KEYS, D_KEY // 2)) * (D_KEY // 2) ** -0.5,
        "expert_u": n(ks[13], (DEPTH, N_EXPERTS, D_MODEL)) * D_MODEL ** -0.5,
        "expert_v": n(ks[14], (DEPTH, N_EXPERTS, D_MODEL)) * PEER_HEADS ** -0.5,
        "final_norm_g": 1.0 + 0.02 * n(ks[15], (D_MODEL,)),
    }


def reference(x, norm1_g, w_in, ln_v_g, ln_v_b, w_spatial, b_spatial, out_norm_a_g, out_norm_b_g,
              w_out, norm2_g, w_query, sub_keys, expert_u, expert_v, final_norm_g):
    B, S, _ = x.shape
    cos, sin = rope_tables(S)
    for layer in range(DEPTH):
        h = rms_norm(x, norm1_g[layer])
        proj = h @ w_in[layer]
        u_a, v_a, q, k, v = jnp.split(
            proj, [D_A, 2 * D_A, 2 * D_A + D_B, 2 * D_A + 2 * D_B], axis=-1)
        a_out = spatial_gating(jax.nn.gelu(u_a), jax.nn.gelu(v_a), ln_v_g[layer], ln_v_b[layer],
                               w_spatial[layer], b_spatial[layer])
        q = apply_rope(q.reshape(B, S, B_HEADS, HEAD_DIM), cos, sin)
        k = apply_rope(k.reshape(B, S, B_HEADS, HEAD_DIM), cos, sin)
        v = v.reshape(B, S, B_HEADS, HEAD_DIM)
        b_out = dilated_attention(q, k, v).reshape(B, S, D_B).astype(x.dtype)
        mix = jnp.concatenate([rms_norm(a_out, out_norm_a_g[layer]),
                               rms_norm(b_out, out_norm_b_g[layer])], axis=-1)
        x = x + mix @ w_out[layer]
        x = x + peer(rms_norm(x, norm2_g[layer]), w_query[layer], sub_keys[layer],
                     expert_u[layer], expert_v[layer])
    return rms_norm(x, final_norm_g)
```

```python
import numpy as np
import ml_dtypes
from contextlib import ExitStack
import concourse.bass as bass
import concourse.mybir as mybir
from concourse.bass_utils import run_bass_kernel_spmd

F32 = mybir.dt.float32
BF16 = mybir.dt.bfloat16
U32 = mybir.dt.uint32
U8 = mybir.dt.uint8
ALU = mybir.AluOpType
AF = mybir.ActivationFunctionType
AX = mybir.AxisListType

NCORES = 8
NTOK = 8192
SEQ = 2048
NSEQ = 4
D = 1024
EPS = 1e-6
MOFF = 1920
MW = 3968
TB = 256
JB = 4
TG = 8
DEBUG_STOP = None
PIPE_B8 = True
PV_FULL = True
PIPE_B9 = True
DBG_NSB = 2


class Tr:
    def __init__(self, nc, es):
        self.nc = nc
        self.es = es
        self.streams = {e: [] for e in ("pe", "act", "dve", "sp", "pool")}
        self.semh = {}
        self.cnt = {}
        for e in ("pe", "act", "dve", "pool"):
            self.semh["E" + e] = es.enter_context(nc.semaphore("s_" + e))
            self.cnt["E" + e] = 0
        self.waited = {e: {} for e in self.streams}
        self.lastw = {}
        self.readers = {}

    def _chan(self, name):
        k = "C" + name
        if k not in self.semh:
            self.semh[k] = self.es.enter_context(self.nc.semaphore("c_" + name))
            self.cnt[k] = 0
        return k

    def _waits(self, eng, reads, writes):
        need = {}
        own = "E" + eng

        def add(ev, war=False):
            if ev is None:
                return
            s, v = ev
            if s == own and (war or eng == "pe"):
                return
            if need.get(s, 0) < v:
                need[s] = v
        for k in reads:
            add(self.lastw.get(k))
        for k in writes:
            ev = self.lastw.get(k)
            if ev is not None and ev[0] != own:
                add(ev)
            for r in self.readers.get(k, ()):
                add(r, war=True)
        w = self.waited[eng]
        for s, v in need.items():
            if w.get(s, 0) < v:
                self.streams[eng].append(("w", s, v))
                w[s] = v

    def _commit(self, me, reads, writes):
        for k in writes:
            self.lastw[k] = me
            self.readers[k] = []
        for k in reads:
            self.readers.setdefault(k, []).append(me)

    def op(self, eng, fn, reads=(), writes=()):
        self._waits(eng, reads, writes)
        s = "E" + eng
        self.cnt[s] += 1
        me = (s, self.cnt[s])
        self.streams[eng].append(("o", fn, s, 1))
        self._commit(me, reads, writes)
        return me

    def dma(self, q, chan, out, in_, reads=(), writes=()):
        self._waits(q, reads, writes)
        s = self._chan(chan)
        self.cnt[s] += 16
        me = (s, self.cnt[s])
        self.streams[q].append(("o", lambda e: e.dma_start(out=out, in_=in_), s, 16))
        self._commit(me, reads, writes)
        return me

    def barrier(self):
        for eng in self.streams:
            w = self.waited[eng]
            for s, v in self.cnt.items():
                if v > 0 and w.get(s, 0) < v and not (s == "E" + eng):
                    self.streams[eng].append(("w", s, v))
                    w[s] = v
        self.lastw = {}
        self.readers = {}

    def emit(self, block):
        def replay(name):
            def f(e):
                for it in self.streams[name]:
                    if it[0] == "w":
                        e.wait_ge(self.semh[it[1]], it[2])
                    else:
                        it[1](e).then_inc(self.semh[it[2]], it[3])
            return f
        block.tensor(replay("pe"))
        block.scalar(replay("act"))
        block.vector(replay("dve"))
        block.sync(replay("sp"))
        block.gpsimd(replay("pool"))


class Arena:
    def __init__(self, ap_u8, nbytes):
        self.a = ap_u8
        self.n = nbytes
        self.off = 0

    def alloc(self, shape, dtype):
        esz = {F32: 4, BF16: 2, U32: 4}[dtype]
        n = int(np.prod(shape)) * esz
        n = (n + 63) // 64 * 64
        assert self.off + n <= self.n, ("arena overflow", self.off, n, self.n)
        v = self.a[:, self.off:self.off + n - (n - int(np.prod(shape)) * esz)].bitcast(dtype)
        self.off += n
        if len(shape) == 2:
            v = v.rearrange("p (a b) -> p a b", a=shape[0])
        elif len(shape) == 3:
            v = v.rearrange("p (a b c) -> p a b c", a=shape[0], b=shape[1])
        return v


def build_nc(debug_stop=None):
    nc = bass.Bass("TRN2", target_bir_lowering=False)

    def din(name, shape, dt=F32):
        return nc.dram_tensor(name, list(shape), dt, kind="ExternalInput").ap()
    x_d = din("x", [NTOK, D])
    win_d = din("win", [128, 8, 2560])
    wout_d = din("wout", [128, 8, 1024])
    wq_d = din("wq", [128, 8, 2048])
    g1T_d = din("g1T", [128, 8])
    g2T_d = din("g2T", [128, 8])
    gaT_d = din("gaT", [128, 4])
    gbT_d = din("gbT", [128, 4])
    lng_d = din("lng", [128, 512])
    lnb_d = din("lnb", [128, 512])
    gf_d = din("gf", [128, 1024])
    wsT_d = din("wsT", [128, 8, 128])
    bsT_d = din("bsT", [128, 8])
    kT_d = din("kT", [128, 2, 128])
    ut_d = din("ut", [1024, 16384])
    ev_d = din("ev", [128, 131072])
    identf_d = din("identf", [128, 128])
    identb_d = din("identb", [128, 128], BF16)
    mask_d = din("masktab", [128, MW], BF16)
    cos_d = din("cosT", [128, SEQ])
    sin_d = din("sinT", [128, SEQ])
    iota_d = din("iota", [128, 128])
    out_d = nc.dram_tensor("out", [NTOK, D], F32, kind="ExternalOutput").ap()
    utb_d = nc.dram_tensor("utb", [1024, 16384], BF16, kind="Internal").ap()
    evb_d = nc.dram_tensor("evb", [128, 131072], BF16, kind="Internal").ap()
    x1s_d = nc.dram_tensor("x1s", [NTOK, D], F32, kind="Internal").ap()

    with ExitStack() as es:
        ARENA_BYTES = 211968
        arena_t = es.enter_context(nc.sbuf_tensor("arena", [128, ARENA_BYTES], U8))
        AR = Arena(arena_t, ARENA_BYTES)
        psb = [es.enter_context(nc.psum_tensor("ps%d" % i, [128, 512], F32)) for i in range(8)]

        def PS(i):
            return psb[i][:]
        T = Tr(nc, es)

        def mm(out, lhsT, rhs, start, stop, r, w):
            T.op("pe", lambda e: e.matmul(out, lhsT, rhs, start=start, stop=stop), r, w)

        def tr(out, in_, ident, r, w):
            T.op("pe", lambda e: e.transpose(out, in_, ident), r, w)

        def act(out, in_, func, r, w, bias=None, scale=None, accum=None):
            kw = {}
            if bias is not None:
                kw["bias"] = bias
            if scale is not None:
                kw["scale"] = scale
            if accum is not None:
                kw["accum_out"] = accum
            T.op("act", lambda e: e.activation(out=out, in_=in_, func=func, **kw), r, w)

        def cp(eng, out, in_, r, w):
            if eng == "act":
                T.op("act", lambda e: e.copy(out, in_), r, w)
            else:
                T.op("dve", lambda e: e.tensor_copy(out, in_), r, w)

        def tt(out, a, b, op, r, w):
            T.op("dve", lambda e: e.tensor_tensor(out=out, in0=a, in1=b, op=op), r, w)

        def ts(out, a, s1, s2, op0, op1, r, w):
            if s2 is None:
                T.op("dve", lambda e: e.tensor_scalar(out=out, in0=a, scalar1=s1, scalar2=None, op0=op0), r, w)
            else:
                T.op("dve", lambda e: e.tensor_scalar(out=out, in0=a, scalar1=s1, scalar2=s2, op0=op0, op1=op1), r, w)

        def stt(out, a, s, b, op0, op1, r, w):
            T.op("dve", lambda e: e.scalar_tensor_tensor(out=out, in0=a, scalar=s, in1=b, op0=op0, op1=op1), r, w)

        def red(out, in_, op, r, w):
            T.op("dve", lambda e: e.tensor_reduce(out=out, in_=in_, axis=AX.X, op=op), r, w)

        def rstd_from_ss(rstd, ss_, n, key_ss, key_rstd):
            act(rstd, ss_, AF.Sqrt, [key_ss], [key_rstd], bias=epsc, scale=1.0 / n)
            T.op("dve", lambda e: e.reciprocal(rstd, rstd), [key_rstd], [key_rstd])

        identf = AR.alloc([128], F32)
        identb = AR.alloc([128], BF16)
        T.dma("sp", "identf", identf, identf_d, [], ["identf"])
        T.dma("sp", "identb", identb, identb_d, [], ["identb"])
        epsc = AR.alloc([1], F32)
        T.op("dve", lambda e: e.memset(epsc, EPS), [], ["epsc"])
        persist_mark = AR.off

        if debug_stop != "A":
            for c in range(8):
                for b in range(4):
                    T.dma("pool", "p0u", utb_d[c * 128:(c + 1) * 128, b * 4096:(b + 1) * 4096],
                          ut_d[c * 128:(c + 1) * 128, b * 4096:(b + 1) * 4096], [], [("utb", c, b)])
            for b in range(32):
                T.dma("pool", "p0v", evb_d[:, b * 4096:(b + 1) * 4096], ev_d[:, b * 4096:(b + 1) * 4096],
                      [], [("evb", b)])

        win = AR.alloc([8, 2048], BF16)
        wout = AR.alloc([8, 1024], BF16)
        hT = AR.alloc([8, SEQ], BF16)
        mixT = AR.alloc([8, SEQ], BF16)
        Vall = AR.alloc([16, 8 * 66], BF16)
        bout = AR.alloc([16, 512], BF16)
        QTz = [AR.alloc([SEQ], BF16) for _ in range(2)]
        KT = AR.alloc([SEQ], BF16)
        masktab = AR.alloc([MW], BF16)
        cs = [[AR.alloc([512], F32) for _ in range(2)] for _ in range(2)]
        PT = [AR.alloc([512], BF16) for _ in range(3)]
        xt = [AR.alloc([1024], F32) for _ in range(2)]
        stage = [AR.alloc([1024], F32)] * 2
        f512 = [AR.alloc([512], F32) for _ in range(4)]
        b1024 = AR.alloc([1024], BF16)
        b512 = [AR.alloc([512], BF16) for _ in range(2)]
        lng = AR.alloc([512], F32)
        lnb = AR.alloc([512], F32)
        wsT = AR.alloc([8, 128], BF16)
        small = AR.alloc([64], F32)
        g1T = AR.alloc([8], F32)
        gaT = AR.alloc([4], F32)
        gbT = AR.alloc([4], F32)
        bsT = AR.alloc([8], F32)
        print("phase A arena bytes", AR.off)

        for nm, dst, src in (("masktab", masktab, mask_d), ("lng", lng, lng_d), ("lnb", lnb, lnb_d),
                             ("g1T", g1T, g1T_d), ("gaT", gaT, gaT_d), ("gbT", gbT, gbT_d), ("bsT", bsT, bsT_d)):
            T.dma("sp", nm, dst, src, [], [nm])

        stg_n = [0]

        def load_conv(dst, src, ncols, key_w, neg=False):
            for c0 in range(0, ncols, 1024):
                c1 = min(ncols, c0 + 1024)
                s = 0
                T.dma("sp", "stage%d" % s, stage[s][:, 0:c1 - c0], src[:, c0:c1], [], [("stage", s)])
                if neg:
                    T.op("act", lambda e, o=dst[:, c0:c1], i=stage[s][:, 0:c1 - c0]: e.mul(o, i, -1.0),
                         [("stage", s)], [key_w])
                else:
                    cp("dve", dst[:, c0:c1], stage[s][:, 0:c1 - c0], [("stage", s)], [key_w])

        for c in range(8):
            load_conv(wout[:, c, :], wout_d[:, c, :], 1024, "wout")
        load_conv(wsT.rearrange("p a b -> p (a b)"), wsT_d.rearrange("p a b -> p (a b)"), 1024, "wsT")
        Vall4 = Vall.rearrange("p t (h c) -> p t h c", c=66)
        T.op("dve", lambda e: e.memset(Vall4[:, :, :, 64:66], 1.0), [], ["Vones"])
        for e2 in range(2):
            T.op("dve", lambda e, o=QTz[e2]: e.memset(o, 0.0), [], [("QK", 0, g_) for g_ in range(4)])

        def load_winA():
            for c in range(8):
                load_conv(win[:, c, 0:1024], win_d[:, c, 0:1024], 1024, "win")
                load_conv(win[:, c, 1024:1536], win_d[:, c, 2048:2560], 512, "win")

        def load_winQ():
            for c in range(8):
                load_conv(win[:, c, 0:1024], win_d[:, c, 1024:2048], 1024, "win")
                src4 = win_d[:, c, 1024:2048].rearrange("p (h t c) -> p h t c", t=2, c=32)
                dst4 = win[:, c, 1024:2048].rearrange("p (h t c) -> p h t c", t=2, c=32)
                s = 0
                st4 = stage[s][:, 0:1024].rearrange("p (h t c) -> p h t c", t=2, c=32)
                T.dma("sp", "stage%d" % s, stage[s][:, 0:1024], win_d[:, c, 1024:2048], [], [("stage", s)])
                T.op("act", lambda e, o=dst4[:, :, 0, :], i=st4[:, :, 1, :]: e.mul(o, i, -1.0),
                     [("stage", s)], ["win"])
                cp("dve", dst4[:, :, 1, :], st4[:, :, 0, :], [("stage", s)], ["win"])

        ss = small[:, 0:1]
        rs = small[:, 1:2]
        mean = small[:, 2:3]
        rl = small[:, 4:8]

        for sq in range(0 if debug_stop == "B" else (NSEQ if debug_stop != "A" else 1)):
            tok0 = sq * SEQ
            load_winA()
            for i in range(16):
                s = i % 2
                rows = slice(tok0 + i * 128, tok0 + (i + 1) * 128)
                T.dma("sp", "xt%d" % s, xt[s], x_d[rows, :], [], [("xt", s)])
                sqj = stage[0][:, 0:1024]
                act(sqj, xt[s], AF.Square, [("xt", s)], [("stage", 0), "ss"], accum=ss)
                rstd_from_ss(rs, ss, 1024.0, "ss", "rs")
                ts(b1024, xt[s], rs, None, ALU.mult, None, [("xt", s), "rs"], ["b1024"])
                pt = PS(7).bitcast(BF16)
                for c in range(8):
                    tr(pt[:, c * 128:(c + 1) * 128], b1024[:, c * 128:(c + 1) * 128], identb,
                       ["b1024", "identb"], [("ps", 7)])
                tt(hT[:, :, i * 128:(i + 1) * 128], pt.rearrange("p (c t) -> p c t", c=8),
                   g1T.unsqueeze(2).to_broadcast([128, 8, 128]), ALU.mult,
                   [("ps", 7), "g1T"], [("hT", i)])
            for i in range(16):
                tsl = slice(i * 128, (i + 1) * 128)
                for (bank, c0) in ((0, 0), (1, 512), (2, 1024)):
                    for c in range(8):
                        mm(PS(bank), hT[:, c, tsl], win[:, c, c0:c0 + 512], c == 0, c == 7,
                           [("hT", i), "win"], [("ps", bank)])
                gu, gv, cen, az = f512
                act(gu, PS(0), AF.Gelu_apprx_tanh, [("ps", 0)], ["f0"])
                act(gv, PS(1), AF.Gelu_apprx_tanh, [("ps", 1)], ["f1", "ss"], accum=ss)
                T.op("act", lambda e, o=Vall4[:, i, :, 0:64], a=PS(2).rearrange("p (h c) -> p h c", c=64): e.copy(o, a),
                     [("ps", 2)], [("Vall", i)])
                ts(mean, ss, 1.0 / 512, None, ALU.mult, None, ["ss"], ["mean"])
                ts(cen, gv, mean, None, ALU.subtract, None, ["f1", "mean"], ["f2"])
                act(az, cen, AF.Square, ["f2"], ["f3", "ss"], accum=ss)
                rstd_from_ss(rs, ss, 512.0, "ss", "rs")
                stt(cen, cen, rs, lng, ALU.mult, ALU.mult, ["f2", "rs", "lng"], ["f2"])
                tt(b512[0], cen, lnb, ALU.add, ["f2", "lnb"], ["vln"])
                for g in range(8):
                    mm(PS(3)[:, g * 64:(g + 1) * 64], wsT[:, g, :], b512[0][:, g * 64:(g + 1) * 64], True, True,
                       ["wsT", "vln"], [("ps", 3)])
                tt(az.rearrange("p (g c) -> p g c", c=64), PS(3).rearrange("p (g c) -> p g c", c=64),
                   bsT.unsqueeze(2).to_broadcast([128, 8, 64]), ALU.add, [("ps", 3), "bsT"], ["f3"])
                tt(az, az, gu, ALU.mult, ["f3", "f0"], ["f3"])
                act(cen, az, AF.Square, ["f3"], ["f2", "ss"], accum=ss)
                rstd_from_ss(rs, ss, 512.0, "ss", "rs")
                ts(b512[1], az, rs, None, ALU.mult, None, ["f3", "rs"], ["an"])
                pt = PS(7).bitcast(BF16)
                for c in range(4):
                    tr(pt[:, c * 128:(c + 1) * 128], b512[1][:, c * 128:(c + 1) * 128], identb,
                       ["an", "identb"], [("ps", 7)])
                tt(mixT[:, 0:4, tsl], pt[:, 0:512].rearrange("p (c t) -> p c t", c=4),
                   gaT.unsqueeze(2).to_broadcast([128, 4, 128]), ALU.mult, [("ps", 7), "gaT"], [("mixA", i)])
            load_winQ()
            hg = 0
            for hp in range(4):
                for g in range(4):
                    gs = slice(g * 512, (g + 1) * 512)
                    s = g % 2
                    T.dma("sp", "cos%d" % s, cs[s][0], cos_d[:, gs], [], [("cos", s)])
                    T.dma("sp", "sin%d" % s, cs[s][1], sin_d[:, gs], [], [("sin", s)])
                    for qk, dstT in ((0, None), (1, KT)):
                        cbase = qk * 512 + hp * 128
                        for (bank, cb) in ((0, cbase), (1, 1024 + cbase)):
                            for c in range(8):
                                mm(PS(bank), win[:, c, cb:cb + 128], hT[:, c, gs], c == 0, c == 7,
                                   ["win"] + [("hT", 4 * g + k) for k in range(4)], [("ps", bank)])
                        t0, t1 = f512[0], f512[1]
                        tt(t0, PS(0), cs[s][0], ALU.mult, [("ps", 0), ("cos", s)], ["f0"])
                        tt(t1, PS(1), cs[s][1], ALU.mult, [("ps", 1), ("sin", s)], ["f1"])
                        if qk == 1:
                            tt(dstT[:, gs], t0, t1, ALU.add, ["f0", "f1"], [("QK", qk, g)])
                        else:
                            for e2 in range(2):
                                pr = slice(64 * e2, 64 * e2 + 64)
                                tt(QTz[e2][pr, gs], t0[pr, :], t1[pr, :], ALU.add, ["f0", "f1"], [("QK", qk, g)])
                for e2 in range(2):
                    h = 2 * hp + e2
                    prt = slice(64 * e2, 64 * e2 + 64)
                    for g in range(4):
                        q0 = g * 512
                        kts = [kt for kt in range(16)
                               if (q0 - kt * 128 + 511 >= -1024) and (q0 - kt * 128 - 127 <= 1024)]
                        sbanks = (2, 3, 7)
                        PT4 = PT + [b1024[:, 0:512]]
                        PK4 = [("PT", 0), ("PT", 1), ("PT", 2), "b1024"]

                        def att_s1(n, kt):
                            sb_ = sbanks[n % 3]
                            mm(PS(sb_), KT[:, kt * 128:(kt + 1) * 128], QTz[e2][:, q0:q0 + 512], True, True,
                               [("QK", 1, kt // 4), ("QK", 0, g)], [("ps", sb_)])
                            p = PT4[n % 4]
                            pk = PK4[n % 4]
                            act(p, PS(sb_), AF.Exp, [("ps", sb_)], [pk], scale=0.125)
                            mo = q0 - kt * 128 + MOFF
                            tt(p, p, masktab[:, mo:mo + 512], ALU.mult, [pk, "masktab"], [pk])

                        accb = 4 + (hg % 2)
                        oT = f512[2 + (hg % 2)]
                        okey = "f%d" % (2 + (hg % 2))

                        def att_s2(n, kt, accb=accb):
                            p = PT4[n % 4]
                            pk = PK4[n % 4]
                            vflat = Vall.rearrange("p t c -> p (t c)")
                            v0 = kt * 528 + h * 66
                            if v0 + 128 <= 16 * 528 and PV_FULL:
                                mm(PS(accb)[:, :], vflat[:, v0:v0 + 128], p, n == 0, n == len(kts) - 1,
                                   [pk, ("Vall", kt), ("Vall", min(kt + 1, 15)), "Vones"], [("ps", accb)])
                            else:
                                mm(PS(accb)[0:65, :], Vall4[:, kt, h, 0:65], p, n == 0, n == len(kts) - 1,
                                   [pk, ("Vall", kt), "Vones"], [("ps", accb)])
                        att_s1(0, kts[0])
                        att_s1(1, kts[1])
                        for n, kt in enumerate(kts):
                            if n + 2 < len(kts):
                                att_s1(n + 2, kts[n + 2])
                            att_s2(n, kt)
                        cp("dve", oT[0:65, :], PS(accb)[0:65, :], [("ps", accb)], [okey])
                        for qt in range(4):
                            tr(PS(6)[:, qt * 65:(qt + 1) * 65], oT[0:65, qt * 128:(qt + 1) * 128], identf[0:65, 0:65],
                               [okey, "identf"], [("ps", 6)])
                        po3 = PS(6)[:, 0:260].rearrange("p (q c) -> p q c", c=65)
                        T.op("dve", lambda e, o=rl, a=po3[:, :, 64]: e.reciprocal(o, a), [("ps", 6)], ["rl"])
                        tt(bout[:, 4 * g:4 * g + 4, h * 64:(h + 1) * 64], po3[:, :, 0:64],
                           rl.unsqueeze(2).to_broadcast([128, 4, 64]), ALU.mult,
                           [("ps", 6), "rl"], [("bout", 4 * g + k) for k in range(4)])
                        hg += 1
            def s4_a(i):
                tsl = slice(i * 128, (i + 1) * 128)
                act(f512[0], bout[:, i, :], AF.Square, [("bout", i)], ["f0", "ss"], accum=ss)
                rstd_from_ss(rs, ss, 512.0, "ss", "rs")
                ts(b512[1], bout[:, i, :], rs, None, ALU.mult, None, [("bout", i), "rs"], ["an"])
                pt = PS(7).bitcast(BF16)
                for c in range(4):
                    tr(pt[:, c * 128:(c + 1) * 128], b512[1][:, c * 128:(c + 1) * 128], identb,
                       ["an", "identb"], [("ps", 7)])
                tt(mixT[:, 4:8, tsl], pt[:, 0:512].rearrange("p (c t) -> p c t", c=4),
                   gbT.unsqueeze(2).to_broadcast([128, 4, 128]), ALU.mult, [("ps", 7), "gbT"], [("mixB", i)])

            def s4_b(i):
                tsl = slice(i * 128, (i + 1) * 128)
                rows = slice(tok0 + i * 128, tok0 + (i + 1) * 128)
                s = i % 2
                T.dma("sp", "xt%d" % s, xt[s], x_d[rows, :], [], [("xt", s)])
                for half in range(2):
                    for c in range(8):
                        mm(PS(half), mixT[:, c, tsl], wout[:, c, half * 512:(half + 1) * 512], c == 0, c == 7,
                           [("mixA", i), ("mixB", i), "wout"], [("ps", half)])
                    tt(xt[s][:, half * 512:(half + 1) * 512], PS(half), xt[s][:, half * 512:(half + 1) * 512],
                       ALU.add, [("ps", half), ("xt", s)], [("xt", s)])
                dst = out_d if debug_stop == "A" else x1s_d
                T.dma("pool", "x1o%d" % s, dst[rows, :], xt[s], [("xt", s)], [("x1s", tok0 // 128 + i)])
            s4_a(0)
            s4_a(1)
            for i in range(16):
                if i + 2 < 16:
                    s4_a(i + 2)
                s4_b(i)
        T.barrier()
        AR.off = persist_mark

        if debug_stop != "A":
            W = AR.alloc([TB, 128], BF16)
            UTs = [AR.alloc([8, JB * 128], BF16) for _ in range(2)]
            Vs = [AR.alloc([JB, 1024], BF16) for _ in range(2)]
            wq = AR.alloc([8, 2048], BF16)
            XnT = AR.alloc([8, TB], BF16)
            XnT2 = AR.alloc([8, TB], BF16)
            x1p = AR.alloc([1024], F32)
            QTp = AR.alloc([16, TB], BF16)
            S = AR.alloc([16, 128], F32)
            Qrep = [AR.alloc([TG, 128], BF16) for _ in range(2)]
            Bm = [AR.alloc([TG, 128], BF16) for _ in range(2)]
            Et = [AR.alloc([4, 128], F32) for _ in range(2)]
            At = [AR.alloc([4, 128], BF16) for _ in range(2)]
            Gt = [AR.alloc([TB], BF16) for _ in range(2)]
            Wg = [AR.alloc([TB], BF16) for _ in range(2)]
            x1t = AR.alloc([1024], F32)
            top = AR.alloc([16, 16], F32)
            idx = AR.alloc([8, 16], U32)
            cand = AR.alloc([8, 256], F32)
            cand2 = AR.alloc([8, 256], F32)
            c16 = AR.alloc([8, 16], F32)
            tokm = AR.alloc([3, 128], F32)
            TT3 = AR.alloc([3, TB], F32)
            gf = AR.alloc([1024], F32)
            kTb = AR.alloc([2, 128], BF16)
            iota = AR.alloc([128], F32)
            g2T = AR.alloc([8], F32)
            sm = AR.alloc([64], F32)
            xnb = AR.alloc([1024], BF16)
            stage = [cand.rearrange("p a b -> p (a b)"), cand2.rearrange("p a b -> p (a b)")]
            print("phase B arena bytes", AR.off)
            for nm, dst, src in (("gf", gf, gf_d), ("iota", iota, iota_d), ("g2T", g2T, g2T_d)):
                T.dma("sp", nm, dst, src, [], [nm])
            for c in range(8):
                load_conv(wq[:, c, :], wq_d[:, c, :], 2048, "wq")
            load_conv(kTb.rearrange("p a b -> p (a b)"), kT_d.rearrange("p a b -> p (a b)"), 256, "kTb")
            ss = sm[:, 0:1]
            rs = sm[:, 1:2]
            c17 = sm[:, 8:16]
            c16m = sm[:, 16:24]
            tau = sm[:, 24:32]
            Z = sm[:, 32:40]
            tl = sm[:, 40:48]
            invZ = sm[:, 48:56]
            top4 = top.rearrange("p (h t) r -> p h t r", t=2)
            utb_v = utb_d.rearrange("(c p) n -> p c n", p=128)
            nblk = 128 // JB
            stream_n = [0]

            def issue_stream(jb):
                s = stream_n[0] % 2
                stream_n[0] += 1
                T.dma("sp", "uts%d" % s, UTs[s].rearrange("p c (j i) -> p c j i", j=JB),
                      utb_v[:, :, jb * JB * 128:(jb + 1) * JB * 128].rearrange("p c (j i) -> p c j i", j=JB),
                      [], [("UTs", s)])
                T.dma("sp", "vs%d" % s, Vs[s].rearrange("p j d -> p (j d)"),
                      evb_d[:, jb * JB * 1024:(jb + 1) * JB * 1024], [], [("Vs", s)])
                return s

            x1src = x_d if debug_stop == "B" else x1s_d
            XnTs = [XnT, XnT2]

            def prep_gen(sb):
                t0 = sb * TB
                XnT_ = XnTs[sb % 2]
                kx = ("XnT", sb % 2)
                for a in range(2):
                    T.dma("sp", "x1p", x1p, x1src[t0 + a * 128:t0 + (a + 1) * 128, :],
                          [("x1s", t0 // 128 + a)], ["x1p"])
                    act(stage[0][:, 0:1024], x1p, AF.Square, ["x1p"], ["cand", "ss"], accum=ss)
                    rstd_from_ss(rs, ss, 1024.0, "ss", "rs")
                    ts(xnb, x1p, rs, None, ALU.mult, None, ["x1p", "rs"], ["xnb"])
                    pt = PS(7).bitcast(BF16)
                    for c in range(8):
                        tr(pt[:, c * 128:(c + 1) * 128], xnb[:, c * 128:(c + 1) * 128], identb,
                           ["xnb", "identb"], [("ps", 7)])
                    tt(XnT_[:, :, a * 128:(a + 1) * 128], pt.rearrange("p (c t) -> p c t", c=8),
                       g2T.unsqueeze(2).to_broadcast([128, 8, 128]), ALU.mult, [("ps", 7), "g2T"], [kx])
                    yield
                for g2 in range(8):
                    bank = 7
                    for k in range(2):
                        g16 = 2 * g2 + k
                        for c in range(8):
                            mm(PS(bank)[:, k * 256:(k + 1) * 256], wq[:, c, g16 * 128:(g16 + 1) * 128], XnT_[:, c, :],
                               c == 0, c == 7, ["wq", kx], [("ps", bank)])
                    cp("act" if g2 % 2 else "dve", QTp[:, 2 * g2:2 * g2 + 2, :],
                       PS(bank).rearrange("p (k t) -> p k t", k=2), [("ps", bank)], ["QTp"])
                    yield
                for a in range(2):
                    asl = slice(a * 128, (a + 1) * 128)
                    for gq in range(4):
                        bank = 7
                        for k in range(4):
                            g16 = gq * 4 + k
                            mm(PS(bank)[:, k * 128:(k + 1) * 128], QTp[:, g16, asl], kTb[:, g16 % 2, :], True, True,
                               ["QTp", "kTb"], [("ps", bank)])
                        cp("act" if gq % 2 else "dve", S[:, gq * 4:gq * 4 + 4, :],
                           PS(bank).rearrange("p (k t) -> p k t", k=4), [("ps", bank)], ["S"])
                        yield
                    S16 = cand2.rearrange("p a b -> p (a b)").rearrange("p (g k) -> p g k", g=16)
                    for g16 in range(16):
                        T.op("dve", lambda e, o=top[:, g16, 0:8], i=S[:, g16, :]: e.max(out=o, in_=i), ["S"], [("top", g16)])
                        if g16 % 4 == 3:
                            yield
                    for g16 in range(1, 16, 2):
                        T.op("dve", lambda e, o=idx[:, g16 // 2, 0:8], m=top[:, g16, 0:8], i=S[:, g16, :]:
                             e.max_index(out=o, in_max=m, in_values=i), ["S", ("top", g16)], [("idx", g16)])
                    yield
                    for g16 in range(16):
                        T.op("dve", lambda e, o=S16[:, g16, :], m=top[:, g16, 0:8], i=S[:, g16, :]:
                             e.match_replace(out=o, in_to_replace=m, in_values=i, imm_value=-1e30),
                             ["S", ("top", g16)], [("S16", g16), "cand2"])
                        if g16 % 4 == 3:
                            yield
                    yield
                    for g16 in range(16):
                        T.op("dve", lambda e, o=top[:, g16, 8:16], i=S16[:, g16, :]: e.max(out=o, in_=i),
                             [("S16", g16)], [("top", g16)])
                        if g16 % 4 == 3:
                            yield
                    for g16 in range(1, 16, 2):
                        T.op("dve", lambda e, o=idx[:, g16 // 2, 8:16], m=top[:, g16, 8:16], i=S16[:, g16, :]:
                             e.max_index(out=o, in_max=m, in_values=i), [("S16", g16), ("top", g16)], [("idx", g16)])
                    yield
                    allS16 = [("S16", g) for g in range(16)]
                    alltop = [("top", g) for g in range(16)]
                    allidx = [("idx", g) for g in range(1, 16, 2)]
                    tt(cand.rearrange("p h (a b) -> p h a b", a=16),
                       top4[:, :, 0, :].unsqueeze(3).to_broadcast([128, 8, 16, 16]),
                       top4[:, :, 1, :].unsqueeze(2).to_broadcast([128, 8, 16, 16]), ALU.add, alltop, ["cand"])
                    for h in range(8):
                        T.op("dve", lambda e, o=c16[:, h, 0:8], i=cand[:, h, :]: e.max(out=o, in_=i), ["cand"], [("c16", h)])
                        if h % 4 == 3:
                            yield
                    yield
                    for h in range(8):
                        T.op("dve", lambda e, o=cand2[:, h, :], m=c16[:, h, 0:8], i=cand[:, h, :]:
                             e.match_replace(out=o, in_to_replace=m, in_values=i, imm_value=-1e30),
                             ["cand", ("c16", h)] , [("cand2", h)] + (allS16 if h == 0 else []))
                        if h % 4 == 3:
                            yield
                    for h in range(8):
                        T.op("dve", lambda e, o=c16[:, h, 8:16], i=cand2[:, h, :]: e.max(out=o, in_=i), [("cand2", h)], [("c16", h)])
                        if h % 4 == 3:
                            yield
                    yield
                    for h in range(8):
                        T.op("dve", lambda e, o=cand[:, h, :], m=c16[:, h, 8:16], i=cand2[:, h, :]:
                             e.match_replace(out=o, in_to_replace=m, in_values=i, imm_value=-1e30),
                             [("cand2", h), ("c16", h)], [("cand", h)] + (["cand"] if h == 0 else []))
                        if h % 4 == 3:
                            yield
                    yield
                    allc16 = [("c16", h) for h in range(8)]
                    allcand = [("cand", h) for h in range(8)]
                    red(c17, cand, ALU.max, allcand, ["c17"])
                    red(c16m, c16, ALU.min, allc16, ["c16m"])
                    tt(tau, c16m, c17, ALU.add, ["c16m", "c17"], ["tau"])
                    ts(tau, tau, 0.5, None, ALU.mult, None, ["tau"], ["tau"])
                    tt(c16, c16, tau.unsqueeze(2).to_broadcast([128, 8, 16]), ALU.subtract, allc16 + ["tau"], ["c16"] + allc16)
                    yield
                    act(c16, c16, AF.Exp, ["c16"], ["c16"])
                    red(Z, c16, ALU.add, ["c16"], ["Z"])
                    T.op("dve", lambda e: e.reciprocal(invZ, Z), ["Z"], ["invZ"])
                    act(Z, Z, AF.Ln, ["Z"], ["Z"])
                    tt(tl, tau, Z, ALU.add, ["tau", "Z"], ["tl"])
                    yield
                    tm3 = tokm.rearrange("p k (h r) -> p k h r", h=8)
                    tt(tm3[:, 0], top4[:, :, 1, :], tl.unsqueeze(2).to_broadcast([128, 8, 16]), ALU.subtract,
                       alltop + ["tl"], ["tokm"])
                    cp("dve", tm3[:, 1], invZ.unsqueeze(2).to_broadcast([128, 8, 16]), ["invZ"], ["tokm"])
                    cp("dve", tm3[:, 2], idx, allidx, ["tokm"])
                    for k in range(3):
                        tr(PS(7)[:, k * 128:(k + 1) * 128], tokm[:, k, :], identf, ["tokm", "identf"], [("ps", 7)])
                    cp("act", TT3[:, :, asl], PS(7)[:, 0:384].rearrange("p (k t) -> p k t", k=3), [("ps", 7)], ["TT3"])
                    yield

            def run_b8(sb):
                def b8_group(tg):
                    q_ = Qrep[tg % 2]
                    b_ = Bm[tg % 2]
                    tsl = slice(tg * TG, tg * TG + TG)
                    qsrc = QTp.rearrange("p (h two) t -> p h two t", two=2)[:, :, 0, tsl].rearrange("p h t -> p t h").unsqueeze(3).to_broadcast([128, TG, 8, 16])
                    cp("act", q_.rearrange("p t (h r) -> p t h r", h=8), qsrc, ["QTp"], [("Qrep", tg % 2)])
                    tt(b_, iota.unsqueeze(1).to_broadcast([128, TG, 128]),
                       TT3[:, 2, tsl].unsqueeze(2).to_broadcast([128, TG, 128]), ALU.is_equal, ["iota", "TT3"],
                       [("Bm", tg % 2)])

                def b8_s1(n4):
                    tg, t4 = divmod(n4, TG // 4)
                    if t4 == 0:
                        b8_group(tg)
                    q_ = Qrep[tg % 2]
                    rb = 4 + n4 % 2
                    e_ = Et[n4 % 2]
                    a_ = At[n4 % 2]
                    for k in range(4):
                        tk = t4 * 4 + k
                        mm(PS(rb)[:, k * 128:(k + 1) * 128], q_[:, tk, :], kTb[:, 0, :], True, True,
                           [("Qrep", tg % 2), "kTb"], [("ps", rb)])
                    for k in range(4):
                        tok = n4 * 4 + k
                        act(e_[:, k, :], PS(rb)[:, k * 128:(k + 1) * 128], AF.Exp, [("ps", rb), "TT3"],
                            [("Et", n4 % 2, k)], bias=TT3[:, 0, tok:tok + 1])
                        stt(a_[:, k, :], e_[:, k, :], TT3[:, 1, tok:tok + 1], e_[:, k, :],
                            ALU.is_gt, ALU.mult, ["TT3", ("Et", n4 % 2, k)], [("At", n4 % 2, k)])

                def b8_s2(n4):
                    tg, t4 = divmod(n4, TG // 4)
                    b_ = Bm[tg % 2]
                    wb = 6 + n4 % 2
                    a_ = At[n4 % 2]
                    for k in range(4):
                        tk = t4 * 4 + k
                        mm(PS(wb)[:, k * 128:(k + 1) * 128], a_[:, k, :], b_[:, tk, :], True, True,
                           [("At", n4 % 2, k), ("Bm", tg % 2)], [("ps", wb)])
                    tok = n4 * 4
                    cp("act", W[:, tok:tok + 4, :], PS(wb).rearrange("p (k j) -> p k j", k=4), [("ps", wb)], ["W"])
                NB4 = TB // 4
                b8_s1(0)
                for n4 in range(NB4):
                    if n4 + 1 < NB4:
                        b8_s1(n4 + 1)
                    b8_s2(n4)

            def run_b9(sb, gen):
                XnT_ = XnTs[sb % 2]
                kx = ("XnT", sb % 2)
                slot_of = {}

                def ensure_block(jb):
                    if jb < nblk and jb not in slot_of:
                        slot_of[jb] = issue_stream(jb)

                def b9_s1(j):
                    jb, jj = divmod(j, JB)
                    cur = slot_of[jb]
                    hb = 4 + (j % 3)
                    hs = slice(0, 256)
                    for c in range(8):
                        mm(PS(hb)[:, hs], UTs[cur][:, c, jj * 128:(jj + 1) * 128], XnT_[:, c, :], c == 0, c == 7,
                           [("UTs", cur), kx], [("ps", hb)])

                def b9_s2(j):
                    jb, jj = divmod(j, JB)
                    cur = slot_of[jb]
                    hb = 4 + (j % 3)
                    hs = slice(0, 256)
                    g_ = Gt[j % 2]
                    w_ = Wg[j % 2]
                    act(g_, PS(hb)[:, hs], AF.Gelu_apprx_tanh, [("ps", hb)], [("Gt", j % 2)])
                    tt(w_, g_, W[:, :, j], ALU.mult, [("Gt", j % 2), "W"], [("Wg", j % 2)])
                    for a in range(2):
                        for half in range(2):
                            mm(PS(2 * a + half), w_[:, a * 128:(a + 1) * 128],
                               Vs[cur][:, jj, half * 512:(half + 1) * 512], j == 0, j == 127,
                               [("Wg", j % 2), ("Vs", cur)], [("ps", 2 * a + half)])
                ensure_block(0)
                ensure_block(1)
                b9_s1(0)
                b9_s1(1)
                for j in range(128):
                    if j + 2 < 128:
                        b9_s1(j + 2)
                    b9_s2(j)
                    if j % JB == JB - 1:
                        ensure_block(j // JB + 2)
                    if gen is not None and j >= 2:
                        next(gen, None)
                if gen is not None:
                    for _ in gen:
                        pass

            def run_b10(sb):
                t0 = sb * TB
                for a in range(2):
                    T.dma("sp", "x1t", x1t, x1src[t0 + a * 128:t0 + (a + 1) * 128, :], [("x1s", t0 // 128 + a)], ["x1t"])
                    for half in range(2):
                        hsl = slice(half * 512, (half + 1) * 512)
                        tt(x1t[:, hsl], PS(2 * a + half), x1t[:, hsl], ALU.add,
                           [("ps", 2 * a + half), "x1t"], ["x1t"])
                    act(stage[0][:, 0:1024], x1t, AF.Square, ["x1t"], ["cand", "ss"], accum=ss)
                    rstd_from_ss(rs, ss, 1024.0, "ss", "rs")
                    stt(x1t, x1t, rs, gf, ALU.mult, ALU.mult, ["x1t", "rs", "gf"], ["x1t"])
                    T.dma("pool", "out%d" % a, out_d[t0 + a * 128:t0 + (a + 1) * 128, :], x1t, ["x1t"], [("out", sb, a)])

            nsb = DBG_NSB if debug_stop == "B" else NTOK // TB
            for _ in prep_gen(0):
                pass
            run_b8(0)
            for sb in range(nsb):
                gen = prep_gen(sb + 1) if sb + 1 < nsb else None
                run_b9(sb, gen)
                run_b10(sb)
                if sb + 1 < nsb:
                    run_b8(sb + 1)
        T.barrier()
        block = es.enter_context(nc.Block())
        T.emit(block)
    return nc


def _consts():
    ident = np.eye(128, dtype=np.float32)
    p = np.arange(128)[:, None]
    c = np.arange(MW)[None, :]
    off = c - p - MOFF
    a = np.abs(off)
    M = (a <= 64).astype(np.float32) + ((off % 4 == 0) & (a <= 256)) + ((off % 16 == 0) & (a <= 1024))
    pos = np.arange(SEQ, dtype=np.float32)
    inv = (1.0 / (np.float32(10000.0) ** (np.arange(0, 64, 2, dtype=np.float32) / np.float32(64)))).astype(np.float32)
    ang = (pos[:, None] * inv[None, :]).astype(np.float32)
    r = np.arange(128) % 64 % 32
    cosT = np.cos(ang).astype(np.float32).T[r]
    sinT = np.sin(ang).astype(np.float32).T[r]
    iota = np.broadcast_to(np.arange(128, dtype=np.float32)[None, :], (128, 128)).copy()
    return dict(identf=ident, identb=ident.astype(ml_dtypes.bfloat16), masktab=M.astype(ml_dtypes.bfloat16),
                cosT=np.ascontiguousarray(cosT), sinT=np.ascontiguousarray(sinT), iota=iota)


def _layout(inp):
    f = np.float32

    def chunked(w):
        return np.ascontiguousarray(w.reshape(8, 128, -1).transpose(1, 0, 2)).astype(f)

    def rep(v):
        return np.ascontiguousarray(np.broadcast_to(v[None, :], (128, v.shape[0]))).astype(f)

    def colsT(v, n):
        return np.ascontiguousarray(v.reshape(n, 128).T).astype(f)
    m = {}
    m["win"] = chunked(inp["w_in"][0])
    m["wout"] = chunked(inp["w_out"][0])
    m["wq"] = chunked(inp["w_query"][0])
    m["g1T"] = colsT(inp["norm1_g"][0], 8)
    m["g2T"] = colsT(inp["norm2_g"][0], 8)
    m["gaT"] = colsT(inp["out_norm_a_g"][0], 4)
    m["gbT"] = colsT(inp["out_norm_b_g"][0], 4)
    m["lng"] = rep(inp["ln_v_g"][0])
    m["lnb"] = rep(inp["ln_v_b"][0])
    m["gf"] = rep(inp["final_norm_g"])
    m["wsT"] = np.ascontiguousarray(inp["w_spatial"][0].transpose(2, 0, 1)).astype(f)
    m["bsT"] = np.ascontiguousarray(inp["b_spatial"][0].T).astype(f)
    m["kT"] = np.ascontiguousarray(inp["sub_keys"][0].transpose(2, 0, 1)).astype(f)
    u = inp["expert_u"][0].reshape(128, 128, 1024)
    m["ut"] = np.ascontiguousarray(u.transpose(2, 1, 0)).reshape(1024, 16384).astype(f)
    m["ev"] = np.ascontiguousarray(inp["expert_v"][0].reshape(128, 128 * 1024)).astype(f)
    return m


_NC_CACHE = {}


def kernel(**inputs):
    inp = {k: np.asarray(v) for k, v in inputs.items()}
    shared = _layout(inp)
    shared.update(_consts())
    x = np.ascontiguousarray(inp["x"].reshape(NCORES, NTOK, D)).astype(np.float32)
    key = DEBUG_STOP
    if key not in _NC_CACHE:
        _NC_CACHE[key] = build_nc(DEBUG_STOP)
    nc = _NC_CACHE[key]
    in_maps = []
    for c in range(NCORES):
        mp = dict(shared)
        mp["x"] = x[c]
        in_maps.append(mp)
    res = run_bass_kernel_spmd(nc, in_maps, core_ids=list(range(NCORES)))
    out = np.stack([np.asarray(r["out"]) for r in res.results], axis=0)
    return out.reshape(32, SEQ, D).astype(np.float32)
```

```python
import numpy as np
import ml_dtypes
from contextlib import ExitStack
import concourse.bass as bass
import concourse.mybir as mybir
from concourse.bass_utils import run_bass_kernel_spmd

F32 = mybir.dt.float32
BF16 = mybir.dt.bfloat16
U32 = mybir.dt.uint32
U8 = mybir.dt.uint8
ALU = mybir.AluOpType
AF = mybir.ActivationFunctionType
AX = mybir.AxisListType

NCORES = 8
NTOK = 8192
SEQ = 2048
NSEQ = 4
D = 1024
EPS = 1e-6
MOFF = 1920
MW = 3968
TB = 256
JB = 4
TG = 8
DEBUG_STOP = None
PIPE_B8 = True
STRICT_SAME_ENGINE = True
PV_FULL = True
PIPE_B9 = True
DBG_NSB = 2


class Tr:
    def __init__(self, nc, es):
        self.nc = nc
        self.es = es
        self.streams = {e: [] for e in ("pe", "act", "dve", "sp", "pool")}
        self.semh = {}
        self.cnt = {}
        for e in ("pe", "act", "dve", "pool"):
            self.semh["E" + e] = es.enter_context(nc.semaphore("s_" + e))
            self.cnt["E" + e] = 0
        self.waited = {e: {} for e in self.streams}
        self.lastw = {}
        self.readers = {}

    def _chan(self, name):
        k = "C" + name
        if k not in self.semh:
            self.semh[k] = self.es.enter_context(self.nc.semaphore("c_" + name))
            self.cnt[k] = 0
        return k

    def _waits(self, eng, reads, writes):
        need = {}
        own = "E" + eng

        def add(ev, war=False):
            if ev is None:
                return
            s, v = ev
            if s == own and (eng == "pe" or (war and not STRICT_SAME_ENGINE)):
                return
            if need.get(s, 0) < v:
                need[s] = v
        for k in reads:
            add(self.lastw.get(k))
        for k in writes:
            ev = self.lastw.get(k)
            if ev is not None and (ev[0] != own or STRICT_SAME_ENGINE):
                add(ev)
            for r in self.readers.get(k, ()):
                add(r, war=True)
        w = self.waited[eng]
        for s, v in need.items():
            if w.get(s, 0) < v:
                self.streams[eng].append(("w", s, v))
                w[s] = v

    def _commit(self, me, reads, writes):
        for k in writes:
            self.lastw[k] = me
            self.readers[k] = []
        for k in reads:
            self.readers.setdefault(k, []).append(me)

    def op(self, eng, fn, reads=(), writes=()):
        self._waits(eng, reads, writes)
        s = "E" + eng
        self.cnt[s] += 1
        me = (s, self.cnt[s])
        self.streams[eng].append(("o", fn, s, 1))
        self._commit(me, reads, writes)
        return me

    def dma(self, q, chan, out, in_, reads=(), writes=()):
        self._waits(q, reads, writes)
        s = self._chan(chan)
        self.cnt[s] += 16
        me = (s, self.cnt[s])
        self.streams[q].append(("o", lambda e: e.dma_start(out=out, in_=in_), s, 16))
        self._commit(me, reads, writes)
        return me

    def barrier(self):
        for eng in self.streams:
            w = self.waited[eng]
            for s, v in self.cnt.items():
                if v > 0 and w.get(s, 0) < v and not (s == "E" + eng):
                    self.streams[eng].append(("w", s, v))
                    w[s] = v
        self.lastw = {}
        self.readers = {}

    def emit(self, block):
        def replay(name):
            def f(e):
                for it in self.streams[name]:
                    if it[0] == "w":
                        e.wait_ge(self.semh[it[1]], it[2])
                    else:
                        it[1](e).then_inc(self.semh[it[2]], it[3])
            return f
        block.tensor(replay("pe"))
        block.scalar(replay("act"))
        block.vector(replay("dve"))
        block.sync(replay("sp"))
        block.gpsimd(replay("pool"))


class Arena:
    def __init__(self, ap_u8, nbytes):
        self.a = ap_u8
        self.n = nbytes
        self.off = 0

    def alloc(self, shape, dtype):
        esz = {F32: 4, BF16: 2, U32: 4}[dtype]
        n = int(np.prod(shape)) * esz
        n = (n + 63) // 64 * 64
        assert self.off + n <= self.n, ("arena overflow", self.off, n, self.n)
        v = self.a[:, self.off:self.off + n - (n - int(np.prod(shape)) * esz)].bitcast(dtype)
        self.off += n
        if len(shape) == 2:
            v = v.rearrange("p (a b) -> p a b", a=shape[0])
        elif len(shape) == 3:
            v = v.rearrange("p (a b c) -> p a b c", a=shape[0], b=shape[1])
        return v


def build_nc(debug_stop=None):
    nc = bass.Bass("TRN2", target_bir_lowering=False)

    def din(name, shape, dt=F32):
        return nc.dram_tensor(name, list(shape), dt, kind="ExternalInput").ap()
    x_d = din("x", [NTOK, D])
    win_d = din("win", [128, 8, 2560])
    wout_d = din("wout", [128, 8, 1024])
    wq_d = din("wq", [128, 8, 2048])
    g1T_d = din("g1T", [128, 8])
    g2T_d = din("g2T", [128, 8])
    gaT_d = din("gaT", [128, 4])
    gbT_d = din("gbT", [128, 4])
    lng_d = din("lng", [128, 512])
    lnb_d = din("lnb", [128, 512])
    gf_d = din("gf", [128, 1024])
    wsT_d = din("wsT", [128, 8, 128])
    bsT_d = din("bsT", [128, 8])
    kT_d = din("kT", [128, 2, 128])
    ut_d = din("ut", [1024, 16384])
    ev_d = din("ev", [128, 131072])
    identf_d = din("identf", [128, 128])
    identb_d = din("identb", [128, 128], BF16)
    mask_d = din("masktab", [128, MW], BF16)
    cos_d = din("cosT", [128, SEQ])
    sin_d = din("sinT", [128, SEQ])
    iota_d = din("iota", [128, 128], BF16)
    out_d = nc.dram_tensor("out", [NTOK, D], F32, kind="ExternalOutput").ap()
    utb_d = nc.dram_tensor("utb", [128 // JB, 128, 8, JB * 128], BF16, kind="Internal").ap()
    evb_d = nc.dram_tensor("evb", [128, 131072], BF16, kind="Internal").ap()
    x1s_d = nc.dram_tensor("x1s", [NTOK, D], F32, kind="Internal").ap()

    with ExitStack() as es:
        ARENA_BYTES = 211968
        arena_t = es.enter_context(nc.sbuf_tensor("arena", [128, ARENA_BYTES], U8))
        AR = Arena(arena_t, ARENA_BYTES)
        psb = [es.enter_context(nc.psum_tensor("ps%d" % i, [128, 512], F32)) for i in range(8)]

        def PS(i):
            return psb[i][:]
        T = Tr(nc, es)

        def mm(out, lhsT, rhs, start, stop, r, w):
            T.op("pe", lambda e: e.matmul(out, lhsT, rhs, start=start, stop=stop), r, w)

        def tr(out, in_, ident, r, w):
            T.op("pe", lambda e: e.transpose(out, in_, ident), r, w)

        def act(out, in_, func, r, w, bias=None, scale=None, accum=None):
            kw = {}
            if bias is not None:
                kw["bias"] = bias
            if scale is not None:
                kw["scale"] = scale
            if accum is not None:
                kw["accum_out"] = accum
            T.op("act", lambda e: e.activation(out=out, in_=in_, func=func, **kw), r, w)

        def cp(eng, out, in_, r, w):
            if eng == "act":
                T.op("act", lambda e: e.copy(out, in_), r, w)
            else:
                T.op("dve", lambda e: e.tensor_copy(out, in_), r, w)

        def tt(out, a, b, op, r, w):
            T.op("dve", lambda e: e.tensor_tensor(out=out, in0=a, in1=b, op=op), r, w)

        def ts(out, a, s1, s2, op0, op1, r, w):
            if s2 is None:
                T.op("dve", lambda e: e.tensor_scalar(out=out, in0=a, scalar1=s1, scalar2=None, op0=op0), r, w)
            else:
                T.op("dve", lambda e: e.tensor_scalar(out=out, in0=a, scalar1=s1, scalar2=s2, op0=op0, op1=op1), r, w)

        def stt(out, a, s, b, op0, op1, r, w):
            T.op("dve", lambda e: e.scalar_tensor_tensor(out=out, in0=a, scalar=s, in1=b, op0=op0, op1=op1), r, w)

        def red(out, in_, op, r, w):
            T.op("dve", lambda e: e.tensor_reduce(out=out, in_=in_, axis=AX.X, op=op), r, w)

        def rstd_from_ss(rstd, ss_, n, key_ss, key_rstd):
            act(rstd, ss_, AF.Sqrt, [key_ss], [key_rstd], bias=epsc, scale=1.0 / n)
            T.op("dve", lambda e: e.reciprocal(rstd, rstd), [key_rstd], [key_rstd])

        identf = AR.alloc([128], F32)
        identb = AR.alloc([128], BF16)
        T.dma("sp", "identf", identf, identf_d, [], ["identf"])
        T.dma("sp", "identb", identb, identb_d, [], ["identb"])
        epsc = AR.alloc([1], F32)
        T.op("dve", lambda e: e.memset(epsc, EPS), [], ["epsc"])
        persist_mark = AR.off

        if debug_stop != "A":
            nb_ = 128 // JB
            for c in range(8):
                for b in range(4):
                    j0, j1 = b * nb_ // 4, (b + 1) * nb_ // 4
                    T.dma("pool", "p0u", utb_d[j0:j1, :, c, :].rearrange("j p n -> p j n"),
                          ut_d[c * 128:(c + 1) * 128, j0 * JB * 128:j1 * JB * 128].rearrange("p (j n) -> p j n", n=JB * 128),
                          [], [("utb", c, b)])
            for b in range(32):
                T.dma("pool", "p0v", evb_d[:, b * 4096:(b + 1) * 4096], ev_d[:, b * 4096:(b + 1) * 4096],
                      [], [("evb", b)])

        win = AR.alloc([8, 2048], BF16)
        wout = AR.alloc([8, 1024], BF16)
        hT = AR.alloc([8, SEQ], BF16)
        mixT = AR.alloc([8, SEQ], BF16)
        Vall = AR.alloc([16, 8 * 66], BF16)
        bout = AR.alloc([16, 512], BF16)
        QTz = [AR.alloc([SEQ], BF16) for _ in range(2)]
        KT = AR.alloc([SEQ], BF16)
        masktab = AR.alloc([MW], BF16)
        cs = [[AR.alloc([512], F32) for _ in range(2)] for _ in range(2)]
        PT = [AR.alloc([512], BF16) for _ in range(3)]
        xt = [AR.alloc([1024], F32) for _ in range(2)]
        stage = [AR.alloc([1024], F32)] * 2
        f512 = [AR.alloc([512], F32) for _ in range(4)]
        b1024 = AR.alloc([1024], BF16)
        b512 = [AR.alloc([512], BF16) for _ in range(2)]
        lng = AR.alloc([512], F32)
        lnb = AR.alloc([512], F32)
        wsT = AR.alloc([8, 128], BF16)
        small = AR.alloc([64], F32)
        g1T = AR.alloc([8], F32)
        gaT = AR.alloc([4], F32)
        gbT = AR.alloc([4], F32)
        bsT = AR.alloc([8], F32)
        print("phase A arena bytes", AR.off)

        for nm, dst, src in (("masktab", masktab, mask_d), ("lng", lng, lng_d), ("lnb", lnb, lnb_d),
                             ("g1T", g1T, g1T_d), ("gaT", gaT, gaT_d), ("gbT", gbT, gbT_d), ("bsT", bsT, bsT_d)):
            T.dma("sp", nm, dst, src, [], [nm])

        stg_n = [0]

        def load_conv(dst, src, ncols, key_w, neg=False):
            for c0 in range(0, ncols, 1024):
                c1 = min(ncols, c0 + 1024)
                s = 0
                T.dma("sp", "stage%d" % s, stage[s][:, 0:c1 - c0], src[:, c0:c1], [], [("stage", s)])
                if neg:
                    T.op("act", lambda e, o=dst[:, c0:c1], i=stage[s][:, 0:c1 - c0]: e.mul(o, i, -1.0),
                         [("stage", s)], [key_w])
                else:
                    cp("dve", dst[:, c0:c1], stage[s][:, 0:c1 - c0], [("stage", s)], [key_w])

        for c in range(8):
            load_conv(wout[:, c, :], wout_d[:, c, :], 1024, "wout")
        load_conv(wsT.rearrange("p a b -> p (a b)"), wsT_d.rearrange("p a b -> p (a b)"), 1024, "wsT")
        Vall4 = Vall.rearrange("p t (h c) -> p t h c", c=66)
        T.op("dve", lambda e: e.memset(Vall4[:, :, :, 64:66], 1.0), [], ["Vones"])
        for e2 in range(2):
            T.op("dve", lambda e, o=QTz[e2]: e.memset(o, 0.0), [], [("QK", 0, g_) for g_ in range(4)])

        def load_winA():
            for c in range(8):
                load_conv(win[:, c, 0:1024], win_d[:, c, 0:1024], 1024, "win")
                load_conv(win[:, c, 1024:1536], win_d[:, c, 2048:2560], 512, "win")

        def load_winQ():
            for c in range(8):
                load_conv(win[:, c, 0:1024], win_d[:, c, 1024:2048], 1024, "win")
                src4 = win_d[:, c, 1024:2048].rearrange("p (h t c) -> p h t c", t=2, c=32)
                dst4 = win[:, c, 1024:2048].rearrange("p (h t c) -> p h t c", t=2, c=32)
                s = 0
                st4 = stage[s][:, 0:1024].rearrange("p (h t c) -> p h t c", t=2, c=32)
                T.dma("sp", "stage%d" % s, stage[s][:, 0:1024], win_d[:, c, 1024:2048], [], [("stage", s)])
                T.op("act", lambda e, o=dst4[:, :, 0, :], i=st4[:, :, 1, :]: e.mul(o, i, -1.0),
                     [("stage", s)], ["win"])
                cp("dve", dst4[:, :, 1, :], st4[:, :, 0, :], [("stage", s)], ["win"])

        ss = small[:, 0:1]
        rs = small[:, 1:2]
        mean = small[:, 2:3]
        rl = small[:, 4:8]

        for sq in range(0 if debug_stop == "B" else (NSEQ if debug_stop != "A" else 1)):
            tok0 = sq * SEQ
            load_winA()
            for i in range(16):
                s = i % 2
                rows = slice(tok0 + i * 128, tok0 + (i + 1) * 128)
                T.dma("sp", "xt%d" % s, xt[s], x_d[rows, :], [], [("xt", s)])
                sqj = stage[0][:, 0:1024]
                act(sqj, xt[s], AF.Square, [("xt", s)], [("stage", 0), "ss"], accum=ss)
                rstd_from_ss(rs, ss, 1024.0, "ss", "rs")
                ts(b1024, xt[s], rs, None, ALU.mult, None, [("xt", s), "rs"], ["b1024"])
                pt = PS(7).bitcast(BF16)
                for c in range(8):
                    tr(pt[:, c * 128:(c + 1) * 128], b1024[:, c * 128:(c + 1) * 128], identb,
                       ["b1024", "identb"], [("ps", 7)])
                tt(hT[:, :, i * 128:(i + 1) * 128], pt.rearrange("p (c t) -> p c t", c=8),
                   g1T.unsqueeze(2).to_broadcast([128, 8, 128]), ALU.mult,
                   [("ps", 7), "g1T"], [("hT", i)])
            for i in range(16):
                tsl = slice(i * 128, (i + 1) * 128)
                for (bank, c0) in ((0, 0), (1, 512), (2, 1024)):
                    for c in range(8):
                        mm(PS(bank), hT[:, c, tsl], win[:, c, c0:c0 + 512], c == 0, c == 7,
                           [("hT", i), "win"], [("ps", bank)])
                gu, gv, cen, az = f512
                act(gu, PS(0), AF.Gelu_apprx_tanh, [("ps", 0)], ["f0"])
                act(gv, PS(1), AF.Gelu_apprx_tanh, [("ps", 1)], ["f1", "ss"], accum=ss)
                T.op("act", lambda e, o=Vall4[:, i, :, 0:64], a=PS(2).rearrange("p (h c) -> p h c", c=64): e.copy(o, a),
                     [("ps", 2)], [("Vall", i)])
                ts(mean, ss, 1.0 / 512, None, ALU.mult, None, ["ss"], ["mean"])
                ts(cen, gv, mean, None, ALU.subtract, None, ["f1", "mean"], ["f2"])
                act(az, cen, AF.Square, ["f2"], ["f3", "ss"], accum=ss)
                rstd_from_ss(rs, ss, 512.0, "ss", "rs")
                stt(cen, cen, rs, lng, ALU.mult, ALU.mult, ["f2", "rs", "lng"], ["f2"])
                tt(b512[0], cen, lnb, ALU.add, ["f2", "lnb"], ["vln"])
                for g in range(8):
                    mm(PS(3)[:, g * 64:(g + 1) * 64], wsT[:, g, :], b512[0][:, g * 64:(g + 1) * 64], True, True,
                       ["wsT", "vln"], [("ps", 3)])
                tt(az.rearrange("p (g c) -> p g c", c=64), PS(3).rearrange("p (g c) -> p g c", c=64),
                   bsT.unsqueeze(2).to_broadcast([128, 8, 64]), ALU.add, [("ps", 3), "bsT"], ["f3"])
                tt(az, az, gu, ALU.mult, ["f3", "f0"], ["f3"])
                act(cen, az, AF.Square, ["f3"], ["f2", "ss"], accum=ss)
                rstd_from_ss(rs, ss, 512.0, "ss", "rs")
                ts(b512[1], az, rs, None, ALU.mult, None, ["f3", "rs"], ["an"])
                pt = PS(7).bitcast(BF16)
                for c in range(4):
                    tr(pt[:, c * 128:(c + 1) * 128], b512[1][:, c * 128:(c + 1) * 128], identb,
                       ["an", "identb"], [("ps", 7)])
                tt(mixT[:, 0:4, tsl], pt[:, 0:512].rearrange("p (c t) -> p c t", c=4),
                   gaT.unsqueeze(2).to_broadcast([128, 4, 128]), ALU.mult, [("ps", 7), "gaT"], [("mixA", i)])
            load_winQ()
            hg = 0
            for hp in range(4):
                for g in range(4):
                    gs = slice(g * 512, (g + 1) * 512)
                    s = g % 2
                    T.dma("sp", "cos%d" % s, cs[s][0], cos_d[:, gs], [], [("cos", s)])
                    T.dma("sp", "sin%d" % s, cs[s][1], sin_d[:, gs], [], [("sin", s)])
                    for qk, dstT in ((0, None), (1, KT)):
                        cbase = qk * 512 + hp * 128
                        for (bank, cb) in ((0, cbase), (1, 1024 + cbase)):
                            for c in range(8):
                                mm(PS(bank), win[:, c, cb:cb + 128], hT[:, c, gs], c == 0, c == 7,
                                   ["win"] + [("hT", 4 * g + k) for k in range(4)], [("ps", bank)])
                        t0, t1 = f512[0], f512[1]
                        tt(t0, PS(0), cs[s][0], ALU.mult, [("ps", 0), ("cos", s)], ["f0"])
                        tt(t1, PS(1), cs[s][1], ALU.mult, [("ps", 1), ("sin", s)], ["f1"])
                        if qk == 1:
                            tt(dstT[:, gs], t0, t1, ALU.add, ["f0", "f1"], [("QK", qk, g)])
                        else:
                            for e2 in range(2):
                                pr = slice(64 * e2, 64 * e2 + 64)
                                tt(QTz[e2][pr, gs], t0[pr, :], t1[pr, :], ALU.add, ["f0", "f1"], [("QK", qk, g)])
                for e2 in range(2):
                    h = 2 * hp + e2
                    prt = slice(64 * e2, 64 * e2 + 64)
                    for g in range(4):
                        q0 = g * 512
                        kts = [kt for kt in range(16)
                               if (q0 - kt * 128 + 511 >= -1024) and (q0 - kt * 128 - 127 <= 1024)]
                        sbanks = (2, 3, 7)
                        PT4 = PT + [b1024[:, 0:512]]
                        PK4 = [("PT", 0), ("PT", 1), ("PT", 2), "b1024"]

                        def att_s1(n, kt):
                            sb_ = sbanks[n % 3]
                            mm(PS(sb_), KT[:, kt * 128:(kt + 1) * 128], QTz[e2][:, q0:q0 + 512], True, True,
                               [("QK", 1, kt // 4), ("QK", 0, g)], [("ps", sb_)])
                            p = PT4[n % 4]
                            pk = PK4[n % 4]
                            act(p, PS(sb_), AF.Exp, [("ps", sb_)], [pk], scale=0.125)
                            mo = q0 - kt * 128 + MOFF
                            tt(p, p, masktab[:, mo:mo + 512], ALU.mult, [pk, "masktab"], [pk])

                        accb = 4 + (hg % 2)
                        oT = f512[2 + (hg % 2)]
                        okey = "f%d" % (2 + (hg % 2))

                        def att_s2(n, kt, accb=accb):
                            p = PT4[n % 4]
                            pk = PK4[n % 4]
                            vflat = Vall.rearrange("p t c -> p (t c)")
                            v0 = kt * 528 + h * 66
                            if v0 + 128 <= 16 * 528 and PV_FULL:
                                mm(PS(accb)[:, :], vflat[:, v0:v0 + 128], p, n == 0, n == len(kts) - 1,
                                   [pk, ("Vall", kt), ("Vall", min(kt + 1, 15)), "Vones"], [("ps", accb)])
                            else:
                                mm(PS(accb)[0:65, :], Vall4[:, kt, h, 0:65], p, n == 0, n == len(kts) - 1,
                                   [pk, ("Vall", kt), "Vones"], [("ps", accb)])
                        att_s1(0, kts[0])
                        att_s1(1, kts[1])
                        for n, kt in enumerate(kts):
                            if n + 2 < len(kts):
                                att_s1(n + 2, kts[n + 2])
                            att_s2(n, kt)
                        cp("dve", oT[0:65, :], PS(accb)[0:65, :], [("ps", accb)], [okey])
                        for qt in range(4):
                            tr(PS(6)[:, qt * 65:(qt + 1) * 65], oT[0:65, qt * 128:(qt + 1) * 128], identf[0:65, 0:65],
                               [okey, "identf"], [("ps", 6)])
                        po3 = PS(6)[:, 0:260].rearrange("p (q c) -> p q c", c=65)
                        T.op("dve", lambda e, o=rl, a=po3[:, :, 64]: e.reciprocal(o, a), [("ps", 6)], ["rl"])
                        tt(bout[:, 4 * g:4 * g + 4, h * 64:(h + 1) * 64], po3[:, :, 0:64],
                           rl.unsqueeze(2).to_broadcast([128, 4, 64]), ALU.mult,
                           [("ps", 6), "rl"], [("bout", 4 * g + k) for k in range(4)])
                        hg += 1
            def s4_a(i):
                tsl = slice(i * 128, (i + 1) * 128)
                act(f512[0], bout[:, i, :], AF.Square, [("bout", i)], ["f0", "ss"], accum=ss)
                rstd_from_ss(rs, ss, 512.0, "ss", "rs")
                ts(b512[1], bout[:, i, :], rs, None, ALU.mult, None, [("bout", i), "rs"], ["an"])
                pt = PS(7).bitcast(BF16)
                for c in range(4):
                    tr(pt[:, c * 128:(c + 1) * 128], b512[1][:, c * 128:(c + 1) * 128], identb,
                       ["an", "identb"], [("ps", 7)])
                tt(mixT[:, 4:8, tsl], pt[:, 0:512].rearrange("p (c t) -> p c t", c=4),
                   gbT.unsqueeze(2).to_broadcast([128, 4, 128]), ALU.mult, [("ps", 7), "gbT"], [("mixB", i)])

            def s4_b(i):
                tsl = slice(i * 128, (i + 1) * 128)
                rows = slice(tok0 + i * 128, tok0 + (i + 1) * 128)
                s = i % 2
                T.dma("sp", "xt%d" % s, xt[s], x_d[rows, :], [], [("xt", s)])
                for half in range(2):
                    for c in range(8):
                        mm(PS(half), mixT[:, c, tsl], wout[:, c, half * 512:(half + 1) * 512], c == 0, c == 7,
                           [("mixA", i), ("mixB", i), "wout"], [("ps", half)])
                    tt(xt[s][:, half * 512:(half + 1) * 512], PS(half), xt[s][:, half * 512:(half + 1) * 512],
                       ALU.add, [("ps", half), ("xt", s)], [("xt", s)])
                dst = out_d if debug_stop == "A" else x1s_d
                T.dma("pool", "x1o%d" % s, dst[rows, :], xt[s], [("xt", s)], [("x1s", tok0 // 128 + i)])
            s4_a(0)
            s4_a(1)
            for i in range(16):
                if i + 2 < 16:
                    s4_a(i + 2)
                s4_b(i)
        T.barrier()
        AR.off = persist_mark

        if debug_stop != "A":
            W = AR.alloc([TB, 128], BF16)
            UTs = [AR.alloc([8, JB * 128], BF16) for _ in range(2)]
            Vs = [AR.alloc([JB, 1024], BF16) for _ in range(2)]
            wq = AR.alloc([8, 2048], BF16)
            XnT = AR.alloc([8, TB], BF16)
            XnT2 = AR.alloc([8, TB], BF16)
            x1p = AR.alloc([1024], F32)
            QTp = AR.alloc([16, TB], BF16)
            S = AR.alloc([16, 128], F32)
            Qrep = [AR.alloc([TG, 128], BF16) for _ in range(2)]
            Bm = [AR.alloc([TG, 128], BF16) for _ in range(2)]
            Et = [AR.alloc([4, 128], F32) for _ in range(2)]
            At = [AR.alloc([4, 128], BF16) for _ in range(2)]
            Gt = [AR.alloc([TB], BF16) for _ in range(2)]
            Wg = [AR.alloc([TB], BF16) for _ in range(2)]
            x1t = AR.alloc([1024], F32)
            top = AR.alloc([16, 16], F32)
            idx = AR.alloc([8, 16], U32)
            cand = AR.alloc([8, 256], F32)
            cand2 = AR.alloc([8, 256], F32)
            c16 = AR.alloc([8, 16], F32)
            tokm = AR.alloc([3, 128], F32)
            TT3 = AR.alloc([3, TB], F32)
            gf = AR.alloc([1024], F32)
            kTb = AR.alloc([2, 128], BF16)
            iota = AR.alloc([128], BF16)
            idxb = AR.alloc([TB], BF16)
            g2T = AR.alloc([8], F32)
            sm = AR.alloc([64], F32)
            xnb = AR.alloc([1024], BF16)
            stage = [cand.rearrange("p a b -> p (a b)"), cand2.rearrange("p a b -> p (a b)")]
            print("phase B arena bytes", AR.off)
            for nm, dst, src in (("gf", gf, gf_d), ("iota", iota, iota_d), ("g2T", g2T, g2T_d)):
                T.dma("sp", nm, dst, src, [], [nm])
            for c in range(8):
                load_conv(wq[:, c, :], wq_d[:, c, :], 2048, "wq")
            load_conv(kTb.rearrange("p a b -> p (a b)"), kT_d.rearrange("p a b -> p (a b)"), 256, "kTb")
            ss = sm[:, 0:1]
            rs = sm[:, 1:2]
            c17 = sm[:, 8:16]
            c16m = sm[:, 16:24]
            tau = sm[:, 24:32]
            Z = sm[:, 32:40]
            tl = sm[:, 40:48]
            invZ = sm[:, 48:56]
            top4 = top.rearrange("p (h t) r -> p h t r", t=2)
            nblk = 128 // JB
            stream_n = [0]

            def issue_stream(jb):
                s = stream_n[0] % 2
                stream_n[0] += 1
                T.dma("sp", "uts%d" % s, UTs[s], utb_d[jb], [], [("UTs", s)])
                T.dma("sp", "vs%d" % s, Vs[s].rearrange("p j d -> p (j d)"),
                      evb_d[:, jb * JB * 1024:(jb + 1) * JB * 1024], [], [("Vs", s)])
                return s

            x1src = x_d if debug_stop == "B" else x1s_d
            XnTs = [XnT, XnT2]

            def prep_gen(sb):
                t0 = sb * TB
                XnT_ = XnTs[sb % 2]
                kx = ("XnT", sb % 2)
                for a in range(2):
                    T.dma("sp", "x1p", x1p, x1src[t0 + a * 128:t0 + (a + 1) * 128, :],
                          [("x1s", t0 // 128 + a)], ["x1p"])
                    act(stage[0][:, 0:1024], x1p, AF.Square, ["x1p"], ["cand", "ss"], accum=ss)
                    rstd_from_ss(rs, ss, 1024.0, "ss", "rs")
                    ts(xnb, x1p, rs, None, ALU.mult, None, ["x1p", "rs"], ["xnb"])
                    pt = PS(7).bitcast(BF16)
                    for c in range(8):
                        tr(pt[:, c * 128:(c + 1) * 128], xnb[:, c * 128:(c + 1) * 128], identb,
                           ["xnb", "identb"], [("ps", 7)])
                    tt(XnT_[:, :, a * 128:(a + 1) * 128], pt.rearrange("p (c t) -> p c t", c=8),
                       g2T.unsqueeze(2).to_broadcast([128, 8, 128]), ALU.mult, [("ps", 7), "g2T"], [kx])
                    yield
                for g2 in range(8):
                    bank = 7
                    for k in range(2):
                        g16 = 2 * g2 + k
                        for c in range(8):
                            mm(PS(bank)[:, k * 256:(k + 1) * 256], wq[:, c, g16 * 128:(g16 + 1) * 128], XnT_[:, c, :],
                               c == 0, c == 7, ["wq", kx], [("ps", bank)])
                    cp("act" if g2 % 2 else "dve", QTp[:, 2 * g2:2 * g2 + 2, :],
                       PS(bank).rearrange("p (k t) -> p k t", k=2), [("ps", bank)], ["QTp"])
                    yield
                for a in range(2):
                    asl = slice(a * 128, (a + 1) * 128)
                    for gq in range(4):
                        bank = 7
                        for k in range(4):
                            g16 = gq * 4 + k
                            mm(PS(bank)[:, k * 128:(k + 1) * 128], QTp[:, g16, asl], kTb[:, g16 % 2, :], True, True,
                               ["QTp", "kTb"], [("ps", bank)])
                        cp("act" if gq % 2 else "dve", S[:, gq * 4:gq * 4 + 4, :],
                           PS(bank).rearrange("p (k t) -> p k t", k=4), [("ps", bank)], ["S"])
                        yield
                    S16 = cand2.rearrange("p a b -> p (a b)").rearrange("p (g k) -> p g k", g=16)
                    for g16 in range(16):
                        T.op("dve", lambda e, o=top[:, g16, 0:8], i=S[:, g16, :]: e.max(out=o, in_=i), ["S"], [("top", g16)])
                        if g16 % 4 == 3:
                            yield
                    for g16 in range(1, 16, 2):
                        T.op("dve", lambda e, o=idx[:, g16 // 2, 0:8], m=top[:, g16, 0:8], i=S[:, g16, :]:
                             e.max_index(out=o, in_max=m, in_values=i), ["S", ("top", g16)], [("idx", g16)])
                    yield
                    for g16 in range(16):
                        T.op("dve", lambda e, o=S16[:, g16, :], m=top[:, g16, 0:8], i=S[:, g16, :]:
                             e.match_replace(out=o, in_to_replace=m, in_values=i, imm_value=-1e30),
                             ["S", ("top", g16)], [("S16", g16), "cand2"])
                        if g16 % 4 == 3:
                            yield
                    yield
                    for g16 in range(16):
                        T.op("dve", lambda e, o=top[:, g16, 8:16], i=S16[:, g16, :]: e.max(out=o, in_=i),
                             [("S16", g16)], [("top", g16)])
                        if g16 % 4 == 3:
                            yield
                    for g16 in range(1, 16, 2):
                        T.op("dve", lambda e, o=idx[:, g16 // 2, 8:16], m=top[:, g16, 8:16], i=S16[:, g16, :]:
                             e.max_index(out=o, in_max=m, in_values=i), [("S16", g16), ("top", g16)], [("idx", g16)])
                    yield
                    allS16 = [("S16", g) for g in range(16)]
                    alltop = [("top", g) for g in range(16)]
                    allidx = [("idx", g) for g in range(1, 16, 2)]
                    tt(cand.rearrange("p h (a b) -> p h a b", a=16),
                       top4[:, :, 0, :].unsqueeze(3).to_broadcast([128, 8, 16, 16]),
                       top4[:, :, 1, :].unsqueeze(2).to_broadcast([128, 8, 16, 16]), ALU.add, alltop, ["cand"])
                    for h in range(8):
                        T.op("dve", lambda e, o=c16[:, h, 0:8], i=cand[:, h, :]: e.max(out=o, in_=i), ["cand"], [("c16", h)])
                        if h % 4 == 3:
                            yield
                    yield
                    for h in range(8):
                        T.op("dve", lambda e, o=cand2[:, h, :], m=c16[:, h, 0:8], i=cand[:, h, :]:
                             e.match_replace(out=o, in_to_replace=m, in_values=i, imm_value=-1e30),
                             ["cand", ("c16", h)] , [("cand2", h)] + (allS16 if h == 0 else []))
                        if h % 4 == 3:
                            yield
                    for h in range(8):
                        T.op("dve", lambda e, o=c16[:, h, 8:16], i=cand2[:, h, :]: e.max(out=o, in_=i), [("cand2", h)], [("c16", h)])
                        if h % 4 == 3:
                            yield
                    yield
                    for h in range(8):
                        T.op("dve", lambda e, o=cand[:, h, :], m=c16[:, h, 8:16], i=cand2[:, h, :]:
                             e.match_replace(out=o, in_to_replace=m, in_values=i, imm_value=-1e30),
                             [("cand2", h), ("c16", h)], [("cand", h)] + (["cand"] if h == 0 else []))
                        if h % 4 == 3:
                            yield
                    yield
                    allc16 = [("c16", h) for h in range(8)]
                    allcand = [("cand", h) for h in range(8)]
                    red(c17, cand, ALU.max, allcand, ["c17"])
                    red(c16m, c16, ALU.min, allc16, ["c16m"])
                    tt(tau, c16m, c17, ALU.add, ["c16m", "c17"], ["tau"])
                    ts(tau, tau, 0.5, None, ALU.mult, None, ["tau"], ["tau"])
                    tt(c16, c16, tau.unsqueeze(2).to_broadcast([128, 8, 16]), ALU.subtract, allc16 + ["tau"], ["c16"] + allc16)
                    yield
                    act(c16, c16, AF.Exp, ["c16"], ["c16"])
                    red(Z, c16, ALU.add, ["c16"], ["Z"])
                    T.op("dve", lambda e: e.reciprocal(invZ, Z), ["Z"], ["invZ"])
                    act(Z, Z, AF.Ln, ["Z"], ["Z"])
                    tt(tl, tau, Z, ALU.add, ["tau", "Z"], ["tl"])
                    yield
                    tm3 = tokm.rearrange("p k (h r) -> p k h r", h=8)
                    tt(tm3[:, 0], top4[:, :, 1, :], tl.unsqueeze(2).to_broadcast([128, 8, 16]), ALU.subtract,
                       alltop + ["tl"], ["tokm"])
                    cp("dve", tm3[:, 1], invZ.unsqueeze(2).to_broadcast([128, 8, 16]), ["invZ"], ["tokm"])
                    cp("dve", tm3[:, 2], idx, allidx, ["tokm"])
                    for k in range(3):
                        tr(PS(7)[:, k * 128:(k + 1) * 128], tokm[:, k, :], identf, ["tokm", "identf"], [("ps", 7)])
                    cp("act", TT3[:, :, asl], PS(7)[:, 0:384].rearrange("p (k t) -> p k t", k=3), [("ps", 7)], ["TT3"])
                    cp("act", idxb[:, asl], PS(7)[:, 256:384], [("ps", 7)], ["idxb"])
                    yield

            def run_b8(sb):
                def b8_group(tg):
                    q_ = Qrep[tg % 2]
                    b_ = Bm[tg % 2]
                    tsl = slice(tg * TG, tg * TG + TG)
                    qsrc = QTp.rearrange("p (h two) t -> p h two t", two=2)[:, :, 0, tsl].rearrange("p h t -> p t h").unsqueeze(3).to_broadcast([128, TG, 8, 16])
                    cp("act" if tg % 2 else "dve", q_.rearrange("p t (h r) -> p t h r", h=8), qsrc, ["QTp"], [("Qrep", tg % 2)])
                    tt(b_, iota.unsqueeze(1).to_broadcast([128, TG, 128]),
                       idxb[:, tsl].unsqueeze(2).to_broadcast([128, TG, 128]), ALU.is_equal, ["iota", "idxb"],
                       [("Bm", tg % 2)])

                def b8_s1(n4):
                    tg, t4 = divmod(n4, TG // 4)
                    if t4 == 0:
                        b8_group(tg)
                    q_ = Qrep[tg % 2]
                    rb = 4 + n4 % 2
                    e_ = Et[n4 % 2]
                    a_ = At[n4 % 2]
                    for k in range(4):
                        tk = t4 * 4 + k
                        mm(PS(rb)[:, k * 128:(k + 1) * 128], q_[:, tk, :], kTb[:, 0, :], True, True,
                           [("Qrep", tg % 2), "kTb"], [("ps", rb)])
                    for k in range(4):
                        tok = n4 * 4 + k
                        act(e_[:, k, :], PS(rb)[:, k * 128:(k + 1) * 128], AF.Exp, [("ps", rb), "TT3"],
                            [("Et", n4 % 2, k)], bias=TT3[:, 0, tok:tok + 1])
                        stt(a_[:, k, :], e_[:, k, :], TT3[:, 1, tok:tok + 1], e_[:, k, :],
                            ALU.is_gt, ALU.mult, ["TT3", ("Et", n4 % 2, k)], [("At", n4 % 2, k)])

                def b8_s2(n4):
                    tg, t4 = divmod(n4, TG // 4)
                    b_ = Bm[tg % 2]
                    wb = 6 + n4 % 2
                    a_ = At[n4 % 2]
                    for k in range(4):
                        tk = t4 * 4 + k
                        mm(PS(wb)[:, k * 128:(k + 1) * 128], a_[:, k, :], b_[:, tk, :], True, True,
                           [("At", n4 % 2, k), ("Bm", tg % 2)], [("ps", wb)])
                    tok = n4 * 4
                    cp("act", W[:, tok:tok + 4, :], PS(wb).rearrange("p (k j) -> p k j", k=4), [("ps", wb)], ["W"])
                NB4 = TB // 4
                b8_s1(0)
                for n4 in range(NB4):
                    if n4 + 1 < NB4:
                        b8_s1(n4 + 1)
                    b8_s2(n4)

            def run_b9(sb, gen):
                XnT_ = XnTs[sb % 2]
                kx = ("XnT", sb % 2)
                slot_of = {}

                def ensure_block(jb):
                    if jb < nblk and jb not in slot_of:
                        slot_of[jb] = issue_stream(jb)

                def b9_s1(j):
                    jb, jj = divmod(j, JB)
                    cur = slot_of[jb]
                    hb = 4 + (j % 3)
                    hs = slice(0, 256)
                    for c in range(8):
                        mm(PS(hb)[:, hs], UTs[cur][:, c, jj * 128:(jj + 1) * 128], XnT_[:, c, :], c == 0, c == 7,
                           [("UTs", cur), kx], [("ps", hb)])

                def b9_s2(j):
                    jb, jj = divmod(j, JB)
                    cur = slot_of[jb]
                    hb = 4 + (j % 3)
                    hs = slice(0, 256)
                    g_ = Gt[j % 2]
                    w_ = Wg[j % 2]
                    act(g_, PS(hb)[:, hs], AF.Gelu_apprx_tanh, [("ps", hb)], [("Gt", j % 2)])
                    tt(w_, g_, W[:, :, j], ALU.mult, [("Gt", j % 2), "W"], [("Wg", j % 2)])
                    for a in range(2):
                        for half in range(2):
                            mm(PS(2 * a + half), w_[:, a * 128:(a + 1) * 128],
                               Vs[cur][:, jj, half * 512:(half + 1) * 512], j == 0, j == 127,
                               [("Wg", j % 2), ("Vs", cur)], [("ps", 2 * a + half)])
                ensure_block(0)
                ensure_block(1)
                b9_s1(0)
                b9_s1(1)
                for j in range(128):
                    if j + 2 < 128:
                        b9_s1(j + 2)
                    b9_s2(j)
                    if j % JB == JB - 1:
                        ensure_block(j // JB + 2)
                    if gen is not None and j >= 2:
                        next(gen, None)
                if gen is not None:
                    for _ in gen:
                        pass

            def run_b10(sb):
                t0 = sb * TB
                for a in range(2):
                    T.dma("sp", "x1t", x1t, x1src[t0 + a * 128:t0 + (a + 1) * 128, :], [("x1s", t0 // 128 + a)], ["x1t"])
                    for half in range(2):
                        hsl = slice(half * 512, (half + 1) * 512)
                        tt(x1t[:, hsl], PS(2 * a + half), x1t[:, hsl], ALU.add,
                           [("ps", 2 * a + half), "x1t"], ["x1t"])
                    act(stage[0][:, 0:1024], x1t, AF.Square, ["x1t"], ["cand", "ss"], accum=ss)
                    rstd_from_ss(rs, ss, 1024.0, "ss", "rs")
                    stt(x1t, x1t, rs, gf, ALU.mult, ALU.mult, ["x1t", "rs", "gf"], ["x1t"])
                    T.dma("pool", "out%d" % a, out_d[t0 + a * 128:t0 + (a + 1) * 128, :], x1t, ["x1t"], [("out", sb, a)])

            nsb = DBG_NSB if debug_stop == "B" else NTOK // TB
            for _ in prep_gen(0):
                pass
            run_b8(0)
            for sb in range(nsb):
                gen = prep_gen(sb + 1) if sb + 1 < nsb else None
                run_b9(sb, gen)
                run_b10(sb)
                if sb + 1 < nsb:
                    run_b8(sb + 1)
        T.barrier()
        block = es.enter_context(nc.Block())
        T.emit(block)
    return nc


def _consts():
    ident = np.eye(128, dtype=np.float32)
    p = np.arange(128)[:, None]
    c = np.arange(MW)[None, :]
    off = c - p - MOFF
    a = np.abs(off)
    M = (a <= 64).astype(np.float32) + ((off % 4 == 0) & (a <= 256)) + ((off % 16 == 0) & (a <= 1024))
    pos = np.arange(SEQ, dtype=np.float32)
    inv = (1.0 / (np.float32(10000.0) ** (np.arange(0, 64, 2, dtype=np.float32) / np.float32(64)))).astype(np.float32)
    ang = (pos[:, None] * inv[None, :]).astype(np.float32)
    r = np.arange(128) % 64 % 32
    cosT = np.cos(ang).astype(np.float32).T[r]
    sinT = np.sin(ang).astype(np.float32).T[r]
    iota = np.broadcast_to(np.arange(128, dtype=np.float32)[None, :], (128, 128)).astype(ml_dtypes.bfloat16)
    return dict(identf=ident, identb=ident.astype(ml_dtypes.bfloat16), masktab=M.astype(ml_dtypes.bfloat16),
                cosT=np.ascontiguousarray(cosT), sinT=np.ascontiguousarray(sinT), iota=iota)


def _layout(inp):
    f = np.float32

    def chunked(w):
        return np.ascontiguousarray(w.reshape(8, 128, -1).transpose(1, 0, 2)).astype(f)

    def rep(v):
        return np.ascontiguousarray(np.broadcast_to(v[None, :], (128, v.shape[0]))).astype(f)

    def colsT(v, n):
        return np.ascontiguousarray(v.reshape(n, 128).T).astype(f)
    m = {}
    m["win"] = chunked(inp["w_in"][0])
    m["wout"] = chunked(inp["w_out"][0])
    m["wq"] = chunked(inp["w_query"][0])
    m["g1T"] = colsT(inp["norm1_g"][0], 8)
    m["g2T"] = colsT(inp["norm2_g"][0], 8)
    m["gaT"] = colsT(inp["out_norm_a_g"][0], 4)
    m["gbT"] = colsT(inp["out_norm_b_g"][0], 4)
    m["lng"] = rep(inp["ln_v_g"][0])
    m["lnb"] = rep(inp["ln_v_b"][0])
    m["gf"] = rep(inp["final_norm_g"])
    m["wsT"] = np.ascontiguousarray(inp["w_spatial"][0].transpose(2, 0, 1)).astype(f)
    m["bsT"] = np.ascontiguousarray(inp["b_spatial"][0].T).astype(f)
    m["kT"] = np.ascontiguousarray(inp["sub_keys"][0].transpose(2, 0, 1)).astype(f)
    u = inp["expert_u"][0].reshape(128, 128, 1024)
    m["ut"] = np.ascontiguousarray(u.transpose(2, 1, 0)).reshape(1024, 16384).astype(f)
    m["ev"] = np.ascontiguousarray(inp["expert_v"][0].reshape(128, 128 * 1024)).astype(f)
    return m


_NC_CACHE = {}


def kernel(**inputs):
    inp = {k: np.asarray(v) for k, v in inputs.items()}
    shared = _layout(inp)
    shared.update(_consts())
    x = np.ascontiguousarray(inp["x"].reshape(NCORES, NTOK, D)).astype(np.float32)
    key = DEBUG_STOP
    if key not in _NC_CACHE:
        _NC_CACHE[key] = build_nc(DEBUG_STOP)
    nc = _NC_CACHE[key]
    in_maps = []
    for c in range(NCORES):
        mp = dict(shared)
        mp["x"] = x[c]
        in_maps.append(mp)
    res = run_bass_kernel_spmd(nc, in_maps, core_ids=list(range(NCORES)))
    out = np.stack([np.asarray(r["out"]) for r in res.results], axis=0)
    return out.reshape(32, SEQ, D).astype(np.float32)
```
